# Optimizing a Trainium2 kernel written in Bass

```python
import math
import jax, jax.numpy as jnp
from jax import lax
import numpy as np

D_MODEL = 1024
BATCH = 16
SEQ = 2048
DEPTH = 1

N_Q_HEADS = 8
N_KV_HEADS = 2
HEAD_DIM = 64
WINDOW = 128
BLOCK = 128
ATT_WIDTH = N_Q_HEADS * HEAD_DIM
KV_WIDTH = N_KV_HEADS * HEAD_DIM
GLA_HEADS = 4
GLA_DK = 64
GLA_DV = 128
GLA_K_WIDTH = GLA_HEADS * GLA_DK
GLA_V_WIDTH = GLA_HEADS * GLA_DV
GLA_GATE_RANK = 16
GLA_TAU = 16.0
GLA_CHUNK = 64
REL_BUCKETS = 32
REL_MAX_DIST = 128
D_FF = 2816
EPS = 1e-6
IN_SPLITS = (ATT_WIDTH, KV_WIDTH, KV_WIDTH, GLA_K_WIDTH, GLA_K_WIDTH, GLA_V_WIDTH, GLA_V_WIDTH, GLA_GATE_RANK, D_MODEL, D_MODEL)
IN_WIDTH = sum(IN_SPLITS)

kernel_name = "hybrid_swa_sink_gla_macaron_block"


def rmsnorm(x, g):
    x32 = x.astype(jnp.float32)
    y = x32 * lax.rsqrt(jnp.mean(x32 * x32, axis=-1, keepdims=True) + EPS)
    return (y * g.astype(jnp.float32)).astype(x.dtype)


def swiglu(x, w_gate, w_up, w_down):
    return (jax.nn.silu(x @ w_gate) * (x @ w_up)) @ w_down


def t5_causal_bucket(dist):
    n = jnp.clip(dist, 0, REL_MAX_DIST - 1)
    max_exact = REL_BUCKETS // 2
    nf = jnp.maximum(n, 1).astype(jnp.float32)
    large = max_exact + (jnp.log(nf / max_exact) / math.log(REL_MAX_DIST / max_exact)
                         * (REL_BUCKETS - max_exact)).astype(jnp.int32)
    large = jnp.minimum(large, REL_BUCKETS - 1)
    return jnp.where(n < max_exact, n, large)


def sliding_window_attention(q, k, v, sinks, rel_table):
    B, S = q.shape[0], q.shape[1]
    nb = S // BLOCK
    G = N_Q_HEADS // N_KV_HEADS
    qb = q.reshape(B, nb, BLOCK, N_KV_HEADS, G, HEAD_DIM)

    def band(t):
        tb = t.reshape(B, nb, BLOCK, N_KV_HEADS, HEAD_DIM)
        prev = jnp.pad(tb[:, :-1], ((0, 0), (1, 0), (0, 0), (0, 0), (0, 0)))
        return jnp.concatenate([prev, tb], axis=2)

    kb, vb = band(k), band(v)
    logits = jnp.einsum('bnqkgd,bnskd->bkgnqs', qb, kb).astype(jnp.float32) * (HEAD_DIM ** -0.5)
    t_idx = jnp.arange(BLOCK)[:, None]
    j_idx = jnp.arange(2 * BLOCK)[None, :]
    dist = t_idx + BLOCK - j_idx
    bias = rel_table.astype(jnp.float32)[t5_causal_bucket(dist)]
    bias = bias.transpose(2, 0, 1).reshape(N_KV_HEADS, G, 1, BLOCK, 2 * BLOCK)
    key_pos = (jnp.arange(nb)[:, None, None] - 1) * BLOCK + j_idx[None]
    valid = (dist >= 0)[None] & (dist < WINDOW)[None] & (key_pos >= 0)
    logits = jnp.where(valid, logits + bias, -1e30)
    s = sinks.astype(jnp.float32).reshape(N_KV_HEADS, G, 1, 1, 1)
    m = jnp.maximum(jnp.max(logits, axis=-1, keepdims=True), s)
    p = jnp.exp(logits - m)
    denom = jnp.sum(p, axis=-1, keepdims=True) + jnp.exp(s - m)
    probs = (p / denom).astype(v.dtype)
    out = jnp.einsum('bkgnqs,bnskd->bnqkgd', probs, vb)
    return out.reshape(B, S, ATT_WIDTH)


def gated_linear_attention(q, k, v, log_a):
    B, S = q.shape[0], q.shape[1]
    nc = S // GLA_CHUNK

    def chunks(t):
        return t.reshape(B, nc, GLA_CHUNK, GLA_HEADS, -1).transpose(0, 3, 1, 2, 4).astype(jnp.float32)

    qc = chunks(q) * (GLA_DK ** -0.5)
    kc, vc, la = chunks(k), chunks(v), chunks(log_a)
    b = jnp.cumsum(la, axis=3)
    b_last = b[:, :, :, -1:, :]
    q_dec = qc * jnp.exp(b)
    k_intra = kc * jnp.exp(-b)
    k_state = kc * jnp.exp(b_last - b)
    causal = jnp.tril(jnp.ones((GLA_CHUNK, GLA_CHUNK), dtype=bool))
    attn = jnp.where(causal, jnp.einsum('bhntd,bhnsd->bhnts', q_dec, k_intra), 0.0)
    o_intra = jnp.einsum('bhnts,bhnse->bhnte', attn, vc)
    d_state = jnp.einsum('bhnsd,bhnse->bhnde', k_state, vc)
    decay = jnp.exp(b_last[:, :, :, 0, :])

    def step(state, inp):
        d, ds = inp
        return d[..., None] * state + ds, state

    s0 = jnp.zeros((B, GLA_HEADS, GLA_DK, GLA_DV), jnp.float32)
    _, s_prev = lax.scan(step, s0, (jnp.moveaxis(decay, 2, 0), jnp.moveaxis(d_state, 2, 0)))
    s_prev = jnp.moveaxis(s_prev, 0, 2)
    o = o_intra + jnp.einsum('bhntd,bhnde->bhnte', q_dec, s_prev)
    return o.transpose(0, 2, 3, 1, 4).reshape(B, S, GLA_HEADS, GLA_DV)


def token_mixing(h, rel_table, w_in, w_alpha, b_alpha, attn_sinks, gla_norm_g, w_proj_a, w_proj_b, w_out):
    B, S = h.shape[0], h.shape[1]
    proj = h @ w_in
    cuts = list(np.cumsum(IN_SPLITS)[:-1])
    q_a, k_a, v_a, q_b, k_b, v_b, r_b, a_lr, g_a, g_b = jnp.split(proj, cuts, axis=-1)
    y_a = sliding_window_attention(q_a.reshape(B, S, N_Q_HEADS, HEAD_DIM),
                                   k_a.reshape(B, S, N_KV_HEADS, HEAD_DIM),
                                   v_a.reshape(B, S, N_KV_HEADS, HEAD_DIM),
                                   attn_sinks, rel_table)
    log_a = jax.nn.log_sigmoid((a_lr @ w_alpha + b_alpha).astype(jnp.float32)) / GLA_TAU
    o_b = gated_linear_attention(q_b.reshape(B, S, GLA_HEADS, GLA_DK),
                                 k_b.reshape(B, S, GLA_HEADS, GLA_DK),
                                 v_b.reshape(B, S, GLA_HEADS, GLA_DV),
                                 log_a.reshape(B, S, GLA_HEADS, GLA_DK))
    o_b = rmsnorm(o_b, gla_norm_g).astype(h.dtype) * jax.nn.silu(r_b.reshape(B, S, GLA_HEADS, GLA_DV))
    y_b = o_b.reshape(B, S, GLA_V_WIDTH)
    merged = jax.nn.sigmoid(g_a) * (y_a.astype(h.dtype) @ w_proj_a) + jax.nn.sigmoid(g_b) * (y_b @ w_proj_b)
    return merged @ w_out


def setup_inputs(seed: int = 0) -> dict:
    key = jax.random.key(seed)
    ks = iter(jax.random.split(key, 32))
    f32 = jnp.float32

    def w(shape, fan_in):
        return jax.random.normal(next(ks), shape, f32) * (fan_in ** -0.5)

    def gain(shape):
        return 1.0 + 0.02 * jax.random.normal(next(ks), shape, f32)

    L = DEPTH
    return {
        "x": jax.random.normal(next(ks), (BATCH, SEQ, D_MODEL), f32),
        "rel_bias": 0.1 * jax.random.normal(next(ks), (REL_BUCKETS, N_Q_HEADS), f32),
        "ffn1_pre_g": gain((L, D_MODEL)),
        "ffn1_w_gate": w((L, D_MODEL, D_FF), D_MODEL),
        "ffn1_w_up": w((L, D_MODEL, D_FF), D_MODEL),
        "ffn1_w_down": w((L, D_FF, D_MODEL), D_FF),
        "ffn1_post_g": gain((L, D_MODEL)),
        "mix_pre_g": gain((L, D_MODEL)),
        "w_in": w((L, D_MODEL, IN_WIDTH), D_MODEL),
        "w_alpha": w((L, GLA_GATE_RANK, GLA_K_WIDTH), GLA_GATE_RANK),
        "b_alpha": 0.1 * jax.random.normal(next(ks), (L, GLA_K_WIDTH), f32),
        "attn_sinks": 0.5 * jax.random.normal(next(ks), (L, N_Q_HEADS), f32),
        "gla_norm_g": gain((L, GLA_DV)),
        "w_proj_a": w((L, ATT_WIDTH, D_MODEL), ATT_WIDTH),
        "w_proj_b": w((L, GLA_V_WIDTH, D_MODEL), GLA_V_WIDTH),
        "w_out": w((L, D_MODEL, D_MODEL), D_MODEL),
        "mix_post_g": gain((L, D_MODEL)),
        "ffn2_pre_g": gain((L, D_MODEL)),
        "ffn2_w_gate": w((L, D_MODEL, D_FF), D_MODEL),
        "ffn2_w_up": w((L, D_MODEL, D_FF), D_MODEL),
        "ffn2_w_down": w((L, D_FF, D_MODEL), D_FF),
        "ffn2_post_g": gain((L, D_MODEL)),
    }


def reference(x, rel_bias, ffn1_pre_g, ffn1_w_gate, ffn1_w_up, ffn1_w_down, ffn1_post_g,
              mix_pre_g, w_in, w_alpha, b_alpha, attn_sinks, gla_norm_g, w_proj_a, w_proj_b,
              w_out, mix_post_g, ffn2_pre_g, ffn2_w_gate, ffn2_w_up, ffn2_w_down, ffn2_post_g):
    for l in range(DEPTH):
        f = swiglu(rmsnorm(x, ffn1_pre_g[l]), ffn1_w_gate[l], ffn1_w_up[l], ffn1_w_down[l])
        x = x + 0.5 * rmsnorm(f, ffn1_post_g[l])
        m = token_mixing(rmsnorm(x, mix_pre_g[l]), rel_bias, w_in[l], w_alpha[l], b_alpha[l],
                         attn_sinks[l], gla_norm_g[l], w_proj_a[l], w_proj_b[l], w_out[l])
        x = x + rmsnorm(m, mix_post_g[l])
        f = swiglu(rmsnorm(x, ffn2_pre_g[l]), ffn2_w_gate[l], ffn2_w_up[l], ffn2_w_down[l])
        x = x + 0.5 * rmsnorm(f, ffn2_post_g[l])
    return x
```

```python
import os
import math
import numpy as np
import concourse.bass as bass
import concourse.mybir as mybir
from concourse.bass_utils import run_bass_kernel_spmd

F32 = mybir.dt.float32
BF16 = mybir.dt.bfloat16
AF = mybir.ActivationFunctionType
ALU = mybir.AluOpType
AX = mybir.AxisListType

D = 1024
DFF = 2816
NJ = 22
T = 512
NTT = 4
NCH = 8
NCORES = 8
EPS = 1e-6
RING = 5
TILE = 4096

GU_T = [("gu%d" % i, 4096) for i in range(11)]
D_T = [("d%d" % i, 4096) for i in range(6)]
MIX_T = [("a0", 4096), ("a1", 4096), ("a2", 4096), ("a3", 4096), ("a4", 8 * 512)] + \
        [("m%d" % m, 3072) for m in range(8)] + [("o0", 4096), ("o1", 4096)]
PASS_TILES = [("f1" + n, u) for n, u in GU_T + D_T] + MIX_T + [("f2" + n, u) for n, u in GU_T + D_T]
NPT = len(PASS_TILES)

C_GAIN = 0
C_GLAG = 48
C_SINK = 49
C_BIAS = 57
C_MASK = C_BIAS + 2048
C_IDENT = C_MASK + 256
C_TRI = C_IDENT + 128
C_CMASK = C_TRI + 128
C_WAUG = C_CMASK + 128
C_EPS = C_WAUG + 256
C_TOT = C_EPS + 1


class Op:
    __slots__ = ("eng", "fn", "deps", "signal", "sigval", "dma", "idx")

    def __init__(self, eng, fn, dma=None):
        self.eng = eng
        self.fn = fn
        self.deps = []
        self.signal = False
        self.sigval = 0
        self.dma = dma


class Tracker:
    ENGS = ("pe", "act", "dve", "pool", "sp")

    def __init__(self):
        self.ops = {e: [] for e in self.ENGS}
        self.segs = {}
        self.dma_count = {}

    def _dep(self, cons, prod, kind):
        if prod is None or prod is cons:
            return
        if prod.dma is None:
            if prod.eng == cons.eng:
                if prod.eng == "pe":
                    return
            prod.signal = True
        cons.deps.append(prod)

    def _touch(self, op, space, lo, hi, write):
        segs = self.segs.setdefault(space, [])
        new = []
        cur = lo
        out = []
        for s in segs:
            if s[1] <= lo or s[0] >= hi:
                out.append(s)
                continue
            if s[0] < lo:
                out.append([s[0], lo, s[2], dict(s[3]), list(s[4])])
                s = [lo, s[1], s[2], s[3], s[4]]
            if s[1] > hi:
                out.append([hi, s[1], s[2], dict(s[3]), list(s[4])])
                s = [s[0], hi, s[2], s[3], s[4]]
            new.append(s)
        new.sort(key=lambda s: s[0])
        filled = []
        for s in new:
            if s[0] > cur:
                filled.append([cur, s[0], None, {}, []])
            filled.append(s)
            cur = s[1]
        if cur < hi:
            filled.append([cur, hi, None, {}, []])
        for s in filled:
            if write:
                self._dep(op, s[2], "waw")
                for r in s[3].values():
                    self._dep(op, r, "war")
                for r in s[4]:
                    self._dep(op, r, "war")
                s[2] = op
                s[3] = {}
                s[4] = []
            else:
                self._dep(op, s[2], "raw")
                if op.dma is not None:
                    s[4].append(op)
                else:
                    s[3][op.eng] = op
        if write:
            filled = [[lo, hi, op, {}, []]]
        out.extend(filled)
        self.segs[space] = out

    def add(self, eng, fn, reads=(), writes=(), dma=None):
        d = None
        if dma is not None:
            self.dma_count[dma] = self.dma_count.get(dma, 0) + 16
            d = [dma, self.dma_count[dma]]
        op = Op(eng, fn, d)
        for (sp, lo, hi) in writes:
            if sp == "ps":
                self._touch(op, sp, lo // 2048 * 2048, (hi + 2047) // 2048 * 2048, True)
            else:
                self._touch(op, sp, lo, hi, True)
        for (sp, lo, hi) in reads:
            if sp == "ps":
                self._touch(op, sp, lo // 2048 * 2048, (hi + 2047) // 2048 * 2048, True)
            else:
                self._touch(op, sp, lo, hi, False)
        self.ops[eng].append(op)
        return op

    def finalize(self):
        for e in self.ENGS:
            k = 0
            for op in self.ops[e]:
                if op.signal and op.dma is None:
                    k += 1
                    op.sigval = k

    def emit(self, eng_name, eng, sems):
        waited = {}
        for op in self.ops[eng_name]:
            need = {}
            for p in op.deps:
                if p.dma is not None:
                    key, val = p.dma[0], p.dma[1]
                else:
                    key, val = "E_" + p.eng, p.sigval
                if val > need.get(key, 0):
                    need[key] = val
            for key, val in need.items():
                if waited.get(key, 0) < val:
                    eng.wait_ge(sems[key], val)
                    waited[key] = val
            ins = op.fn(eng)
            if op.dma is not None:
                ins.then_inc(sems[op.dma[0]], 16)
            elif op.signal:
                ins.then_inc(sems["E_" + eng_name], 1)


class Buf:
    def __init__(self, space, base, off_bytes, dtype, shape, npart=128):
        self.space = space
        self.off = off_bytes
        self.es = 2 if dtype == BF16 else 4
        self.shape = tuple(shape)
        n = int(np.prod(shape))
        self.n = n
        w0 = off_bytes // 4
        w1 = (off_bytes + n * self.es + 3) // 4
        v = base[:, w0:w1]
        if dtype != F32:
            v = v.bitcast(dtype)
        if len(shape) == 2:
            v = v.rearrange("p (a b) -> p a b", a=shape[0])
        elif len(shape) == 3:
            v = v.rearrange("p (a b c) -> p a b c", a=shape[0], b=shape[1])
        self.ap = v

    def r(self, *idx, lo=None, hi=None):
        stride = self.n
        off = 0
        for i, ix in enumerate(idx):
            stride //= self.shape[i]
            off += ix * stride
        a = 0 if lo is None else lo
        b = stride if hi is None else hi
        return (self.space, self.off + (off + a) * self.es, self.off + (off + b) * self.es)


def build_program(stages=("ffn1", "mix", "ffn2"), nch=NCH):
    nc = bass.Bass("TRN2", target_bir_lowering=False)
    xh = nc.dram_tensor("xh", [NCH, 128, 8 * T], F32, kind="ExternalInput").ap()
    wst = nc.dram_tensor("wst", [NPT, 128, TILE], F32, kind="ExternalInput").ap()
    cst = nc.dram_tensor("cst", [128, C_TOT], F32, kind="ExternalInput").ap()
    yh = nc.dram_tensor("yh", [NCH, 128, 8 * T], F32, kind="ExternalOutput").ap()

    SBW = 53000
    sb = nc.alloc_sbuf_tensor("sb", [128, SBW], F32)
    ps = nc.alloc_psum_tensor("ps", [128, 4096], F32)
    tr = Tracker()
    alloc = {"off": 0}

    def sbuf(dtype, shape):
        es = 2 if dtype == BF16 else 4
        n = int(np.prod(shape)) * es
        n = (n + 63) // 64 * 64
        b = Buf("sb", sb, alloc["off"], dtype, shape)
        alloc["off"] += n
        assert alloc["off"] <= SBW * 4, alloc["off"]
        return b

    def psum(bank, off_words, dtype, shape):
        return Buf("ps", ps, (bank * 512 + off_words) * 4, dtype, shape)

    def region(nbytes):
        o = alloc["off"]
        alloc["off"] += (nbytes + 63) // 64 * 64
        assert alloc["off"] <= SBW * 4, alloc["off"]
        return {"base": o, "off": o, "end": o + nbytes}

    def rsub(reg, dtype, shape):
        es = 2 if dtype == BF16 else 4
        n = (int(np.prod(shape)) * es + 63) // 64 * 64
        b = Buf("sb", sb, reg["off"], dtype, shape)
        reg["off"] += n
        assert reg["off"] <= reg["end"], (reg, n)
        return b

    def rreset(reg):
        reg["off"] = reg["base"]

    X = [sbuf(F32, (8, T)) for _ in range(2)]
    hT = sbuf(BF16, (8, T))
    sq = sbuf(BF16, (8, T))
    rstd = sbuf(F32, (T,))
    ring = [sbuf(BF16, (TILE,)) for _ in range(RING)]
    CST = sbuf(F32, (C_TOT,))
    ident = sbuf(BF16, (128,))
    onesA = sbuf(BF16, (128,))
    onesB = sbuf(BF16, (128,))
    nsink = sbuf(F32, (8,))
    gs = [sbuf(BF16, (T,)) for _ in range(2)]
    agc = sbuf(F32, (3, 8))
    kaT = sbuf(BF16, (128 + T,))
    va = sbuf(BF16, (5, 128))
    alrT = sbuf(F32, (T,))
    yaT = sbuf(BF16, (4, T))
    ybT = sbuf(BF16, (4, T))
    pT = [sbuf(BF16, (8, 128)) for _ in range(2)]
    st_mx = [sbuf(F32, (4,)) for _ in range(2)]
    st_negm = [sbuf(F32, (4,)) for _ in range(2)]
    st_rsum = [sbuf(F32, (4,)) for _ in range(2)]
    st_t = [sbuf(F32, (4,)) for _ in range(2)]
    st_es = [sbuf(F32, (4,)) for _ in range(2)]
    st_rden = [sbuf(F32, (4,)) for _ in range(2)]
    e1 = sbuf(F32, (256,))
    spb = sbuf(F32, (256,))
    eb = sbuf(F32, (2, 128))
    enb = sbuf(F32, (2, 128))
    eks = sbuf(F32, (2, 128))
    blast = sbuf(F32, (2, 2))
    decay = sbuf(F32, (2, 2))
    attnT = [sbuf(BF16, (4, 128)) for _ in range(2)]
    S = sbuf(F32, (2, 128))
    Sbf = sbuf(BF16, (8, 2, 128))
    regA = region(NJ * T * 2)
    actT = rsub(regA, BF16, (NJ, T))
    rreset(regA)
    qaT = rsub(regA, BF16, (4, T))
    qbT = rsub(regA, F32, (2, T))
    kbT = rsub(regA, F32, (2, T))
    vb = rsub(regA, BF16, (4, 512))
    rbT = rsub(regA, BF16, (4, T))
    regF = region(8 * T * 4)
    fT = rsub(regF, F32, (8, T))
    rreset(regF)
    oT = rsub(regF, F32, (4, T))
    rstd4 = rsub(regF, F32, (4, T))
    regS = region(16384)
    s_sb = [rsub(regS, F32, (4, 256)) for _ in range(2)]
    p_sb = [rsub(regS, BF16, (4, 256)) for _ in range(2)]
    pn_sb = [rsub(regS, BF16, (4, 256)) for _ in range(2)]
    rreset(regS)
    sga = [rsub(regS, F32, (T,)) for _ in range(2)]
    sgb = [rsub(regS, F32, (T,)) for _ in range(2)]
    t1 = [rsub(regS, F32, (T,)) for _ in range(2)]
    t2 = [rsub(regS, F32, (T,)) for _ in range(2)]
    rreset(regS)
    spb4 = [rsub(regS, F32, (256,)) for _ in range(4)]
    eb4 = [rsub(regS, F32, (2, 128)) for _ in range(4)]
    enb4 = [rsub(regS, F32, (2, 128)) for _ in range(4)]
    eks4 = [rsub(regS, F32, (2, 128)) for _ in range(4)]
    attnT4 = attnT + [sbuf(BF16, (4, 128)) for _ in range(2)]
    blast4 = [sbuf(F32, (2, 2)) for _ in range(4)]
    decay4 = [sbuf(F32, (2, 2)) for _ in range(4)]
    regM = region(8 * T * 2)
    mgT = rsub(regM, BF16, (8, T))
    rreset(regM)
    qdec = rsub(regM, BF16, (2, T))
    kin = rsub(regM, BF16, (2, T))
    kst = rsub(regM, BF16, (2, T))
    kst_tok = rsub(regM, BF16, (4, 256))
    print("SBUF bytes/partition used:", alloc["off"])

    def mm(out_ap, lhsT, rhs, start, stop, reads, writes):
        tr.add("pe", lambda e: e.matmul(out_ap, lhsT, rhs, start=start, stop=stop), reads, writes)

    def tp(out_ap, in_ap, reads, writes):
        tr.add("pe", lambda e: e.transpose(out_ap, in_ap, ident.ap[:, :]), reads + [ident.r()], writes)

    def act(out_ap, in_ap, func, reads, writes, bias=None, scale=None, accum=None):
        kw = {}
        if bias is not None:
            kw["bias"] = bias
        if scale is not None:
            kw["scale"] = scale
        if accum is not None:
            kw["accum_out"] = accum
        tr.add("act", lambda e: e.activation(out_ap, in_ap, func, **kw), reads, writes)

    def dve(fn, reads, writes):
        tr.add("dve", fn, reads, writes)

    tr.add("sp", lambda e: e.dma_start(out=CST.ap, in_=cst), [], [CST.r()], dma="D_cst")
    cview = CST.ap
    dve(lambda e: e.tensor_copy(out=ident.ap, in_=cview[:, C_IDENT:C_IDENT + 128]),
        [CST.r(lo=C_IDENT, hi=C_IDENT + 128)], [ident.r()])
    dve(lambda e: e.memset(onesA.ap, 1.0 / 1024.0), [], [onesA.r()])
    dve(lambda e: e.memset(onesB.ap, 1.0 / 128.0), [], [onesB.r()])
    dve(lambda e: e.tensor_scalar(out=nsink.ap, in0=cview[:, C_SINK:C_SINK + 8], scalar1=-1.0, scalar2=None,
                                  op0=ALU.mult),
        [CST.r(lo=C_SINK, hi=C_SINK + 8)], [nsink.r()])
    biasv = cview[:, C_BIAS:C_BIAS + 2048].rearrange("p (h s) -> p h s", h=8)
    maskv = cview[:, C_MASK:C_MASK + 256]
    dve(lambda e: e.tensor_tensor(out=biasv, in0=biasv, in1=maskv.unsqueeze(1).to_broadcast([128, 8, 256]),
                                  op=ALU.add),
        [CST.r(lo=C_BIAS, hi=C_MASK + 256)], [CST.r(lo=C_BIAS, hi=C_BIAS + 2048)])
    dve(lambda e: e.memset(alrT.ap[0:32, :], 1.0), [], [alrT.r()])
    for i_, (gi_, al_) in enumerate(((1, 0.5), (3, 1.0), (5, 0.5))):
        dve(lambda e, i_=i_, gi_=gi_, al_=al_: e.tensor_scalar(
            out=agc.ap[:, i_, :], in0=cview[:, C_GAIN + gi_ * 8:C_GAIN + gi_ * 8 + 8], scalar1=float(al_), scalar2=None,
            op0=ALU.mult), [CST.r(lo=0, hi=57)], [agc.r(i_)])
    AGI = {1: 0, 3: 1, 5: 2}
    triv = cview[:, C_TRI:C_TRI + 128]
    cmaskv = cview[:, C_CMASK:C_CMASK + 128]
    waugv = cview[0:32, C_WAUG:C_WAUG + 256]
    R_TRI = CST.r(lo=C_TRI, hi=C_TRI + 128)
    R_CMASK = CST.r(lo=C_CMASK, hi=C_CMASK + 128)
    R_WAUG = CST.r(lo=C_WAUG, hi=C_WAUG + 256)
    R_EPS = CST.r(lo=C_EPS, hi=C_EPS + 1)
    epsv = cview[:, C_EPS:C_EPS + 1]

    def gain(n, c):
        return cview[:, C_GAIN + n * 8 + c:C_GAIN + n * 8 + c + 1]

    R_GAIN = CST.r(lo=0, hi=57)

    wstate = {"g": 0}

    def wload(pidx):
        g = wstate["g"]
        wstate["g"] += 1
        slot = g % RING
        used = PASS_TILES[pidx][1]
        b = ring[slot]
        tr.add("pool", lambda e: e.dma_start(out=b.ap[:, 0:used], in_=wst[pidx][:, 0:used]),
               [], [b.r()], dma="D_w%d" % slot)
        return b

    pending = []
    CUT_ = int(os.environ.get("MK_CUT", "99"))
    order = {"pos": 0, "seq": []}
    for ci in range(nch):
        for pidx in range(NPT):
            nm = PASS_TILES[pidx][0]
            if nm.startswith("f1") and "ffn1" not in stages:
                continue
            if nm.startswith("f2") and "ffn2" not in stages:
                continue
            if not (nm.startswith("f1") or nm.startswith("f2")) and "mix" not in stages:
                continue
            if nm[:2] in ("f1", "f2"):
                if CUT_ <= 1:
                    continue
                if CUT_ <= 2 and nm[2] == "d":
                    continue
            order["seq"].append(pidx)

    def wnext(expect_name):
        while len(pending) < RING and order["pos"] < len(order["seq"]):
            pidx = order["seq"][order["pos"]]
            order["pos"] += 1
            pending.append((pidx, wload(pidx)))
        pidx, b = pending.pop(0)
        assert PASS_TILES[pidx][0] == expect_name, (PASS_TILES[pidx][0], expect_name)
        return b

    def wrefill():
        while len(pending) < RING - 1 and order["pos"] < len(order["seq"]):
            pidx = order["seq"][order["pos"]]
            order["pos"] += 1
            pending.append((pidx, wload(pidx)))

    def rms_stats(bank):
        mps = psum(bank, 0, F32, (T,))
        for c in range(8):
            mm(mps.ap, onesA.ap, sq.ap[:, c, :], c == 0, c == 7,
               [onesA.r(), sq.r(c)], [mps.r()])
        act(rstd.ap, mps.ap, AF.Ln, [mps.r(), R_EPS], [rstd.r()], bias=epsv)
        act(rstd.ap, rstd.ap, AF.Exp, [rstd.r()], [rstd.r()], scale=-0.5)

    def prenorm(Xb, gidx, bank=0):
        for c in range(8):
            act(sq.ap[:, c, :], Xb.ap[:, c, :], AF.Square, [Xb.r(c)], [sq.r(c)])
        rms_stats(bank)
        for c in range(8):
            dve(lambda e, c=c: e.scalar_tensor_tensor(out=hT.ap[:, c, :], in0=Xb.ap[:, c, :], scalar=gain(gidx, c),
                                                      in1=rstd.ap, op0=ALU.mult, op1=ALU.mult),
                [Xb.r(c), R_GAIN, rstd.r()], [hT.r(c)])

    def postnorm(Xb, gidx, alpha, fbanks):
        for m in range(8):
            fp = fbanks[m]
            act(sq.ap[:, m, :], fp.ap, AF.Square, [fp.r()], [sq.r(m)])
            dve(lambda e, m=m, fp=fp: e.tensor_copy(out=fT.ap[:, m, :], in_=fp.ap), [fp.r()], [fT.r(m)])

    def postnorm_ops(Xb, bank):
        def stats():
            mps = psum(bank, 0, F32, (T,))
            for c in range(8):
                mm(mps.ap, onesA.ap, sq.ap[:, c, :], c == 0, c == 7, [onesA.r(), sq.r(c)], [mps.r()])
            act(rstd.ap, mps.ap, AF.Ln, [mps.r(), R_EPS], [rstd.r()], bias=epsv)
        ops = [stats,
               lambda: act(rstd.ap, rstd.ap, AF.Exp, [rstd.r()], [rstd.r()], scale=-0.5)]
        for m in range(8):
            ops.append(lambda m=m: dve(lambda e: e.tensor_tensor(out=fT.ap[:, m, :], in0=fT.ap[:, m, :], in1=rstd.ap,
                                                                 op=ALU.mult), [fT.r(m), rstd.r()], [fT.r(m)]))
            ops.append(lambda m=m: dve(lambda e: e.tensor_tensor(out=Xb.ap[:, m, :], in0=Xb.ap[:, m, :], in1=fT.ap[:, m, :],
                                                                 op=ALU.add), [fT.r(m), Xb.r(m)], [Xb.r(m)]))
        return ops

    def postnorm_finish(Xb, gidx, alpha, bank=0):
        for f_ in postnorm_ops(Xb, bank):
            f_()

    CUT = int(os.environ.get("MK_CUT", "99"))

    def ffn(Xb, pfx, g_pre, g_post, skip_prenorm=False, mid_hook=None, drip=None, defer_post=False):
        if CUT <= 0:
            return
        if not skip_prenorm:
            prenorm(Xb, g_pre)
        if CUT <= 1:
            return
        gps = [psum(0, 0, F32, (T,)), psum(1, 0, F32, (T,))]
        ups = [psum(2, 0, F32, (T,)), psum(3, 0, F32, (T,))]
        wt = None
        for j in range(NJ):
            if j % 2 == 0:
                wt = wnext(pfx + "gu%d" % (j // 2))
                wv = wt.ap.rearrange("p (jj gu k c) -> p jj gu k c", jj=2, gu=2, k=8)
            jj = j % 2
            gp, up = gps[j % 2], ups[j % 2]
            if j == 0:
                for k in range(8):
                    for j2 in range(2):
                        mm(gps[j2].ap, wv[:, j2, 0, k, :], hT.ap[:, k, :], k == 0, k == 7, [wt.r(), hT.r(k)], [gps[j2].r()])
                        mm(ups[j2].ap, wv[:, j2, 1, k, :], hT.ap[:, k, :], k == 0, k == 7, [wt.r(), hT.r(k)], [ups[j2].r()])
            elif j >= 2:
                for k in range(8):
                    mm(gp.ap, wv[:, jj, 0, k, :], hT.ap[:, k, :], k == 0, k == 7, [wt.r(), hT.r(k)], [gp.r()])
                for k in range(8):
                    mm(up.ap, wv[:, jj, 1, k, :], hT.ap[:, k, :], k == 0, k == 7, [wt.r(), hT.r(k)], [up.r()])
            g_ = gs[j % 2]
            act(g_.ap, gp.ap, AF.Silu, [gp.r()], [g_.r()])
            dve(lambda e, j=j, g_=g_, up=up: e.tensor_tensor(out=actT.ap[:, j, :], in0=g_.ap, in1=up.ap, op=ALU.mult),
                [g_.r(), up.r()], [actT.r(j)])
            if drip:
                for _ in range(3 if j > 0 else 1):
                    if drip:
                        drip.pop(0)()
            wrefill()
        while drip:
            drip.pop(0)()
        if CUT <= 2:
            return
        if mid_hook is not None:
            mid_hook()
        for half in range(2):
            fps = [psum((4 if half == 0 else 0) + i, 0, F32, (T,)) for i in range(4)]
            for ti in range(3):
                wt = wnext(pfx + "d%d" % (half * 3 + ti))
                wv = wt.ap.rearrange("p (jj n) -> p jj n", jj=8)
                for jj in range(8):
                    j = ti * 8 + jj
                    if j >= NJ:
                        break
                    for i in range(4):
                        mm(fps[i].ap, wv[:, jj, i * 128:(i + 1) * 128], actT.ap[:, j, :], j == 0, j == NJ - 1,
                           [wt.r(), actT.r(j)], [fps[i].r()])
                wrefill()
            for i in range(4):
                m = half * 4 + i
                fp = fps[i]
                act(sq.ap[:, m, :], fp.ap, AF.Square, [fp.r()], [sq.r(m)])
                dve(lambda e, m=m, fp=fp: e.tensor_scalar(out=fT.ap[:, m, :], in0=fp.ap,
                                                          scalar1=agc.ap[:, AGI[g_post], m:m + 1], scalar2=None, op0=ALU.mult),
                    [fp.r(), agc.r()], [fT.r(m)])
        if CUT <= 3:
            return
        if defer_post:
            ops_ = postnorm_ops(Xb, 4)
            ops_.pop(0)()
            return ops_
        postnorm_finish(Xb, g_post, 0.5, bank=4)
        return None

    def proj_fm(wt, wv, col0, ncols, bank, evac):
        pb = psum(bank, 0, F32, (T,))
        for k in range(8):
            mm(pb.ap[0:ncols, :], wv[:, k, col0:col0 + ncols], hT.ap[:, k, :], k == 0, k == 7,
               [wt.r(), hT.r(k)], [pb.r()])
        evac(pb)

    def mixer(Xb, ci):
        first_of_seq = (ci % 4 == 0)
        prenorm(Xb, 2)
        bank = {"i": 0}

        def nb():
            b = bank["i"]
            bank["i"] = (b + 1) % 4
            return b

        wt = wnext("a0")
        wv = wt.ap.rearrange("p (k c) -> p k c", k=8)
        qps = [psum(c, 0, F32, (T,)) for c in range(4)]
        for k in range(8):
            for c in range(4):
                mm(qps[c].ap, wv[:, k, c * 128:(c + 1) * 128], hT.ap[:, k, :], k == 0, k == 7,
                   [wt.r(), hT.r(k)], [qps[c].r()])
        for c in range(4):
            act(qaT.ap[:, c, :], qps[c].ap, AF.Copy, [qps[c].r()], [qaT.r(c)])
        wrefill()
        wt = wnext("a1")
        wv = wt.ap.rearrange("p (k c) -> p k c", k=8)
        proj_fm(wt, wv, 0, 128, nb(),
                lambda pb: act(kaT.ap[:, 128:128 + T], pb.ap, AF.Copy, [pb.r()], [kaT.r(lo=128, hi=128 + T)]))
        pbv = psum(nb(), 0, F32, (4, 128))
        for tt in range(NTT):
            for k in range(8):
                mm(pbv.ap[:, tt, :], hT.ap[:, k, tt * 128:(tt + 1) * 128], wv[:, k, 128:256], k == 0, k == 7,
                   [wt.r(), hT.r(k)], [pbv.r(tt)])
        act(va.ap[:, 1:5, :], pbv.ap, AF.Copy, [pbv.r()], [va.r(lo=128, hi=640)])
        for c in range(2):
            proj_fm(wt, wv, 256 + c * 128, 128, nb(),
                    lambda pb, c=c: dve(lambda e: e.tensor_copy(out=qbT.ap[:, c, :], in_=pb.ap), [pb.r()], [qbT.r(c)]))
        wrefill()
        wt = wnext("a2")
        wv = wt.ap.rearrange("p (k c) -> p k c", k=8)
        for tt in range(NTT):
            pb = psum(nb(), 0, F32, (T,))
            for k in range(8):
                mm(pb.ap, hT.ap[:, k, tt * 128:(tt + 1) * 128], wv[:, k, :], k == 0, k == 7,
                   [wt.r(), hT.r(k)], [pb.r()])
            act(vb.ap[:, tt, :], pb.ap, AF.Copy, [pb.r()], [vb.r(tt)])
        wrefill()
        wt = wnext("a3")
        wv = wt.ap.rearrange("p (k c) -> p k c", k=8)
        for c in range(4):
            proj_fm(wt, wv, c * 128, 128, nb(),
                    lambda pb, c=c: act(rbT.ap[:, c, :], pb.ap, AF.Silu, [pb.r()], [rbT.r(c)]))
        wrefill()
        wt = wnext("a4")
        wv = wt.ap.rearrange("p (k c) -> p k c", k=8)
        for c in range(2):
            proj_fm(wt, wv, c * 128, 128, nb(),
                    lambda pb, c=c: dve(lambda e: e.tensor_copy(out=kbT.ap[:, c, :], in_=pb.ap), [pb.r()], [kbT.r(c)]))
        proj_fm(wt, wv, 256, 16, nb(),
                lambda pb: dve(lambda e: e.tensor_copy(out=alrT.ap[0:16, :], in_=pb.ap[0:16, :]), [pb.r()], [alrT.r()]))
        wrefill()

        H2 = (0, 1)
        spsl = [psum(0, 0, F32, (4, 256)), psum(2, 0, F32, (4, 256))]
        tpsl = [psum(4, 0, BF16, (8, 128)), psum(5, 0, BF16, (8, 128))]
        hsl = [slice(0, 64), slice(64, 128)]
        yps = psum(6, 0, F32, (4, 128))

        def blk_params(tt):
            first_blk = first_of_seq and tt == 0
            ncol = 128 if first_blk else 256
            boff = 128 if first_blk else 0
            kc0 = tt * 128 + boff
            blks = [1] if first_blk else [0, 1]
            return first_blk, ncol, boff, kc0, blks

        def swa_scores(tt):
            first_blk, ncol, boff, kc0, blks = blk_params(tt)
            for half in H2:
                for c in range(4):
                    mm(spsl[half].ap[:, c, 0:ncol], qaT.ap[hsl[half], c, tt * 128:(tt + 1) * 128],
                       kaT.ap[hsl[half], kc0:kc0 + ncol],
                       True, True, [qaT.r(c), kaT.r(lo=kc0, hi=kc0 + ncol)], [spsl[half].r(c)])

        def swa_chain_a(tt):
            first_blk, ncol, boff, kc0, blks = blk_params(tt)
            for half in H2:
                dve(lambda e, sps=spsl[half], s_=s_sb[half], half=half, ncol=ncol, boff=boff: e.scalar_tensor_tensor(
                    out=s_.ap[:, :, 0:ncol], in0=sps.ap[:, :, 0:ncol], scalar=0.125,
                    in1=biasv[:, half * 4:half * 4 + 4, boff:boff + ncol], op0=ALU.mult, op1=ALU.add),
                    [spsl[half].r(), CST.r(lo=C_BIAS, hi=C_BIAS + 2048)], [s_sb[half].r()])
                dve(lambda e, s_=s_sb[half], mx=st_mx[half], ncol=ncol: e.tensor_reduce(
                    out=mx.ap, in_=s_.ap[:, :, 0:ncol], axis=AX.X, op=ALU.max),
                    [s_sb[half].r()], [st_mx[half].r()])
                dve(lambda e, mx=st_mx[half], negm=st_negm[half], half=half: e.scalar_tensor_tensor(
                    out=negm.ap, in0=mx.ap, scalar=-1.0, in1=nsink.ap[:, half * 4:half * 4 + 4],
                    op0=ALU.mult, op1=ALU.min),
                    [st_mx[half].r(), nsink.r()], [st_negm[half].r()])
                dve(lambda e, tt_=st_t[half], negm=st_negm[half], half=half: e.tensor_tensor(
                    out=tt_.ap, in0=negm.ap, in1=cview[:, C_SINK + half * 4:C_SINK + half * 4 + 4], op=ALU.add),
                    [st_negm[half].r(), CST.r(lo=C_SINK, hi=C_SINK + 8)], [st_t[half].r()])
                p_, s_, negm, rsum = p_sb[half], s_sb[half], st_negm[half], st_rsum[half]
                for c in range(4):
                    act(p_.ap[:, c, 0:ncol], s_.ap[:, c, 0:ncol], AF.Exp, [s_.r(c), negm.r()], [p_.r(c), rsum.r()],
                        bias=negm.ap[:, c:c + 1], accum=rsum.ap[:, c:c + 1])
                act(st_es[half].ap, st_t[half].ap, AF.Exp, [st_t[half].r()], [st_es[half].r()])

        def swa_chain_b(tt):
            first_blk, ncol, boff, kc0, blks = blk_params(tt)
            for half in H2:
                dve(lambda e, es=st_es[half], rsum=st_rsum[half]: e.tensor_tensor(out=es.ap, in0=es.ap, in1=rsum.ap, op=ALU.add),
                    [st_es[half].r(), st_rsum[half].r()], [st_es[half].r()])
                dve(lambda e, es=st_es[half], rden=st_rden[half]: e.reciprocal(out=rden.ap, in_=es.ap),
                    [st_es[half].r()], [st_rden[half].r()])
                p_, pn_, rden = p_sb[half], pn_sb[half], st_rden[half]
                for c in range(4):
                    dve(lambda e, p_=p_, pn_=pn_, rden=rden, c=c, ncol=ncol: e.tensor_scalar(
                        out=pn_.ap[:, c, 0:ncol], in0=p_.ap[:, c, 0:ncol], scalar1=rden.ap[:, c:c + 1], scalar2=None,
                        op0=ALU.mult),
                        [p_.r(c), rden.r()], [pn_.r(c)])
            for half in H2:
                tps, pn_ = tpsl[half], pn_sb[half]
                for c in range(4):
                    for bi, blk in enumerate(blks):
                        tp(tps.ap[:, c * 2 + blk, :], pn_.ap[:, c, bi * 128:(bi + 1) * 128],
                           [pn_.r(c)], [tps.r(c * 2 + blk)])
            for half in H2:
                tps, pT_ = tpsl[half], pT[half]
                if first_blk:
                    for c in range(4):
                        dve(lambda e, pT_=pT_, tps=tps, c=c: e.tensor_copy(out=pT_.ap[:, c * 2 + 1, :], in_=tps.ap[:, c * 2 + 1, :]),
                            [tps.r(c * 2 + 1)], [pT_.r(c * 2 + 1)])
                else:
                    dve(lambda e, pT_=pT_, tps=tps: e.tensor_copy(out=pT_.ap, in_=tps.ap), [tps.r()], [pT_.r()])
            for half in H2:
                pT_ = pT[half]
                for c in range(4):
                    for bi, blk in enumerate(blks):
                        mm(yps.ap[hsl[half], c, :], va.ap[:, tt + blk, half * 64:(half + 1) * 64], pT_.ap[:, c * 2 + blk, :],
                           bi == 0, bi == len(blks) - 1,
                           [va.r(tt + blk), pT_.r(c * 2 + blk)], [yps.r(c)])
            act(yaT.ap[:, :, tt * 128:(tt + 1) * 128], yps.ap, AF.Copy, [yps.r()], [yaT.r()])

        swa_scores(0)
        swa_chain_a(0)
        for tt in range(NTT):
            if tt + 1 < NTT:
                swa_scores(tt + 1)
            swa_chain_b(tt)
            if tt + 1 < NTT:
                swa_chain_a(tt + 1)
        dve(lambda e: e.tensor_copy(out=kaT.ap[:, 0:128], in_=kaT.ap[:, T:T + 128]),
            [kaT.r(lo=T, hi=T + 128)], [kaT.r(lo=0, hi=128)])
        dve(lambda e: e.tensor_copy(out=va.ap[:, 0, :], in_=va.ap[:, 4, :]), [va.r(4)], [va.r(0)])

        if first_of_seq:
            dve(lambda e: e.memset(S.ap, 0.0), [], [S.r()])
        TCS = [slice(tt * 128, (tt + 1) * 128) for tt in range(NTT)]
        zps = [psum(tt, 0, F32, (256,)) for tt in range(NTT)]
        bps = [psum(tt, 256, F32, (2, 128)) for tt in range(NTT)]
        trps = [psum(6, tt * 128, BF16, (2, 128)) for tt in range(NTT)]
        dpsl = [psum(4, 0, F32, (2, 128)), psum(5, 0, F32, (2, 128))]
        apsl = [[psum(2 * (tt % 2) + half, 0 if tt < 2 else 256, F32, (2, 128)) for half in range(2)] for tt in range(NTT)]
        opsl = [[psum(4 + 2 * (tt % 2) + half, 0 if tt < 2 else 256, F32, (2, 128)) for half in range(2)] for tt in range(NTT)]
        for tt in range(NTT):
            mm(zps[tt].ap, alrT.ap[0:32, TCS[tt]], waugv, True, True, [alrT.r(), R_WAUG], [zps[tt].r()])
        for tt in range(NTT):
            act(spb4[tt].ap, zps[tt].ap, AF.Exp, [zps[tt].r()], [spb4[tt].r()], scale=-1.0)
            act(spb4[tt].ap, spb4[tt].ap, AF.Ln, [spb4[tt].r()], [spb4[tt].r()], bias=1.0)
        for tt in range(NTT):
            for fc in range(2):
                mm(bps[tt].ap[:, fc, :], spb4[tt].ap[:, fc * 128:(fc + 1) * 128], triv, True, True,
                   [spb4[tt].r(), R_TRI], [bps[tt].r(fc)])
        for tt in range(NTT):
            dve(lambda e, tt=tt: e.tensor_copy(out=blast4[tt].ap, in_=bps[tt].ap[:, :, 63:128:64]),
                [bps[tt].r()], [blast4[tt].r()])
        for tt in range(NTT):
            act(eb4[tt].ap, bps[tt].ap, AF.Exp, [bps[tt].r()], [eb4[tt].r()])
            act(enb4[tt].ap, bps[tt].ap, AF.Exp, [bps[tt].r()], [enb4[tt].r()], scale=-1.0)
            act(decay4[tt].ap, blast4[tt].ap, AF.Exp, [blast4[tt].r()], [decay4[tt].r()])
        for tt in range(NTT):
            tcs = TCS[tt]
            dve(lambda e, tcs=tcs, tt=tt: e.scalar_tensor_tensor(out=qdec.ap[:, :, tcs], in0=qbT.ap[:, :, tcs], scalar=0.125,
                                                                 in1=eb4[tt].ap, op0=ALU.mult, op1=ALU.mult),
                [qbT.r(), eb4[tt].r()], [qdec.r(0, lo=tt * 128, hi=tt * 128 + 128), qdec.r(1, lo=tt * 128, hi=tt * 128 + 128)])
            dve(lambda e, tcs=tcs, tt=tt: e.tensor_tensor(out=kin.ap[:, :, tcs], in0=kbT.ap[:, :, tcs], in1=enb4[tt].ap, op=ALU.mult),
                [kbT.r(), enb4[tt].r()], [kin.r(0, lo=tt * 128, hi=tt * 128 + 128), kin.r(1, lo=tt * 128, hi=tt * 128 + 128)])
            for fc in range(2):
                for sub in range(2):
                    cs_ = slice(tt * 128 + sub * 64, tt * 128 + (sub + 1) * 64)
                    dve(lambda e, tt=tt, fc=fc, sub=sub, cs_=cs_: e.scalar_tensor_tensor(
                        out=kst.ap[:, fc, cs_], in0=kbT.ap[:, fc, cs_], scalar=decay4[tt].ap[:, fc, sub:sub + 1],
                        in1=enb4[tt].ap[:, fc, sub * 64:(sub + 1) * 64], op0=ALU.mult, op1=ALU.mult),
                        [kbT.r(fc), decay4[tt].r(), enb4[tt].r(fc)],
                        [kst.r(fc, lo=tt * 128 + sub * 64, hi=tt * 128 + (sub + 1) * 64)])
        for tt in range(NTT):
            for fc in range(2):
                tp(trps[tt].ap[:, fc, :], kst.ap[:, fc, TCS[tt]], [kst.r(fc, lo=tt * 128, hi=tt * 128 + 128)], [trps[tt].r(fc)])
        for tt in range(NTT):
            act(kst_tok.ap[:, tt, :], trps[tt].ap.rearrange("p a b -> p (a b)"), AF.Copy, [trps[tt].r()], [kst_tok.r(tt)])
        for tt in range(NTT):
            for sub in range(2):
                sc = tt * 2 + sub
                dps = dpsl[sub]
                ss = slice(sub * 64, (sub + 1) * 64)
                for h in range(4):
                    fc, half = h // 2, h % 2
                    mm(dps.ap[half * 64:(half + 1) * 64, fc, :], kst_tok.ap[ss, tt, h * 64:(h + 1) * 64],
                       vb.ap[ss, tt, h * 128:(h + 1) * 128], True, True,
                       [kst_tok.r(tt), vb.r(tt)], [dps.r(fc)])
                dve(lambda e, sc=sc: e.tensor_copy(out=Sbf.ap[:, sc, :, :], in_=S.ap), [S.r()], [Sbf.r(sc)])
                for fc in range(2):
                    dve(lambda e, fc=fc, sub=sub, dps=dps, tt=tt: e.scalar_tensor_tensor(
                        out=S.ap[:, fc, :], in0=S.ap[:, fc, :], scalar=decay4[tt].ap[:, fc, sub:sub + 1],
                        in1=dps.ap[:, fc, :], op0=ALU.mult, op1=ALU.add),
                        [S.r(fc), decay4[tt].r(), dps.r(fc)], [S.r(fc)])
        for tt in range(NTT):
            tcs = TCS[tt]
            for h in range(4):
                fc, half = h // 2, h % 2
                hs = slice(half * 64, (half + 1) * 64)
                mm(apsl[tt][half].ap[:, fc, :], kin.ap[hs, fc, tcs], qdec.ap[hs, fc, tcs], True, True,
                   [kin.r(fc, lo=tt * 128, hi=tt * 128 + 128), qdec.r(fc, lo=tt * 128, hi=tt * 128 + 128)],
                   [apsl[tt][half].r(fc)])
        for tt in range(NTT):
            at_ = attnT4[tt]
            for half in range(2):
                for fc in range(2):
                    h = fc * 2 + half
                    dve(lambda e, half=half, fc=fc, h=h, at_=at_, tt=tt: e.tensor_tensor(
                        out=at_.ap[:, h, :], in0=apsl[tt][half].ap[:, fc, :], in1=cmaskv, op=ALU.mult),
                        [apsl[tt][half].r(fc), R_CMASK], [at_.r(h)])
        for tt in range(NTT):
            at_ = attnT4[tt]
            for h in range(4):
                fc, half = h // 2, h % 2
                hs = slice(half * 64, (half + 1) * 64)
                op_ = opsl[tt][half]
                mm(op_.ap[:, fc, :], vb.ap[:, tt, h * 128:(h + 1) * 128], at_.ap[:, h, :], True, False,
                   [vb.r(tt), at_.r(h)], [op_.r(fc)])
                for sub in range(2):
                    sc = tt * 2 + sub
                    mm(op_.ap[:, fc, sub * 64:(sub + 1) * 64], Sbf.ap[hs, sc, fc, :],
                       qdec.ap[hs, fc, tt * 128 + sub * 64:tt * 128 + (sub + 1) * 64], False, sub == 1,
                       [Sbf.r(sc), qdec.r(fc, lo=tt * 128, hi=tt * 128 + 128)], [op_.r(fc)])
        for tt in range(NTT):
            for h in range(4):
                fc, half = h // 2, h % 2
                act(oT.ap[:, h, TCS[tt]], opsl[tt][half].ap[:, fc, :], AF.Copy, [opsl[tt][half].r(fc)],
                    [oT.r(h, lo=tt * 128, hi=tt * 128 + 128)])
        sq4 = Buf("sb", sb, sq.off, BF16, (4, T))
        for h in range(4):
            act(sq4.ap[:, h, :], oT.ap[:, h, :], AF.Square, [oT.r(h)], [sq.r(h)])
        for h in range(4):
            mps = psum(4 + h % 4, 0, F32, (T,))
            mm(mps.ap, onesB.ap, sq4.ap[:, h, :], True, True, [onesB.r(), sq.r(h)], [mps.r()])
            act(rstd4.ap[:, h, :], mps.ap, AF.Ln, [mps.r(), R_EPS], [rstd4.r(h)], bias=epsv)
            act(rstd4.ap[:, h, :], rstd4.ap[:, h, :], AF.Exp, [rstd4.r(h)], [rstd4.r(h)], scale=-0.5)
            dve(lambda e, h=h: e.scalar_tensor_tensor(out=oT.ap[:, h, :], in0=oT.ap[:, h, :],
                                                      scalar=cview[:, C_GLAG:C_GLAG + 1], in1=rstd4.ap[:, h, :],
                                                      op0=ALU.mult, op1=ALU.mult),
                [oT.r(h), R_GAIN, rstd4.r(h)], [oT.r(h)])
            dve(lambda e, h=h: e.tensor_tensor(out=ybT.ap[:, h, :], in0=oT.ap[:, h, :], in1=rbT.ap[:, h, :], op=ALU.mult),
                [oT.r(h), rbT.r(h)], [ybT.r(h)])

        for m in range(8):
            wt = wnext("m%d" % m)
            wpa = wt.ap[:, 0:512].rearrange("p (c n) -> p c n", c=4)
            wpb = wt.ap[:, 512:1024].rearrange("p (c n) -> p c n", c=4)
            wga = wt.ap[:, 1024:2048].rearrange("p (k n) -> p k n", k=8)
            wgb = wt.ap[:, 2048:3072].rearrange("p (k n) -> p k n", k=8)
            par = m % 2
            pa = psum(0 + par * 4, 0, F32, (T,))
            pbb = psum(1 + par * 4, 0, F32, (T,))
            ga = psum(2 + par * 4, 0, F32, (T,))
            gb = psum(3 + par * 4, 0, F32, (T,))
            for k in range(8):
                mm(ga.ap, wga[:, k, :], hT.ap[:, k, :], k == 0, k == 7, [wt.r(), hT.r(k)], [ga.r()])
            for k in range(8):
                mm(gb.ap, wgb[:, k, :], hT.ap[:, k, :], k == 0, k == 7, [wt.r(), hT.r(k)], [gb.r()])
            for c in range(4):
                mm(pa.ap, wpa[:, c, :], yaT.ap[:, c, :], c == 0, c == 3, [wt.r(), yaT.r(c)], [pa.r()])
            for c in range(4):
                mm(pbb.ap, wpb[:, c, :], ybT.ap[:, c, :], c == 0, c == 3, [wt.r(), ybT.r(c)], [pbb.r()])
            act(sga[par].ap, ga.ap, AF.Sigmoid, [ga.r()], [sga[par].r()])
            act(sgb[par].ap, gb.ap, AF.Sigmoid, [gb.r()], [sgb[par].r()])
            dve(lambda e, par=par, pa=pa: e.tensor_tensor(out=t1[par].ap, in0=sga[par].ap, in1=pa.ap, op=ALU.mult),
                [sga[par].r(), pa.r()], [t1[par].r()])
            dve(lambda e, par=par, pbb=pbb: e.tensor_tensor(out=t2[par].ap, in0=sgb[par].ap, in1=pbb.ap, op=ALU.mult),
                [sgb[par].r(), pbb.r()], [t2[par].r()])
            dve(lambda e, par=par, m=m: e.tensor_tensor(out=mgT.ap[:, m, :], in0=t1[par].ap, in1=t2[par].ap, op=ALU.add),
                [t1[par].r(), t2[par].r()], [mgT.r(m)])
            wrefill()
        for half in range(2):
            wt = wnext("o%d" % half)
            wv = wt.ap.rearrange("p (k n) -> p k n", k=8)
            for i in range(4):
                m = half * 4 + i
                fp = psum(i if half == 0 else 4 + i, 0, F32, (T,))
                for k in range(8):
                    mm(fp.ap, wv[:, k, i * 128:(i + 1) * 128], mgT.ap[:, k, :], k == 0, k == 7,
                       [wt.r(), mgT.r(k)], [fp.r()])
                act(sq.ap[:, m, :], fp.ap, AF.Square, [fp.r()], [sq.r(m)])
                dve(lambda e, m=m, fp=fp: e.tensor_scalar(out=fT.ap[:, m, :], in0=fp.ap,
                                                          scalar1=agc.ap[:, AGI[3], m:m + 1], scalar2=None, op0=ALU.mult),
                    [fp.r(), agc.r()], [fT.r(m)])
            wrefill()
        postnorm_finish(Xb, 3, 1.0)

    def xload(ci):
        Xn = X[ci % 2]
        tr.add("sp", lambda e, ci=ci, Xn=Xn: e.dma_start(out=Xn.ap.rearrange("p a b -> p (a b)"), in_=xh[ci]),
               [], [Xn.r()], dma="D_x%d" % (ci % 2))

    xload(0)
    pre_done = False
    pending_post = None
    for ci in range(nch):
        Xb = X[ci % 2]
        if "ffn1" in stages:
            ffn(Xb, "f1", 0, 1, skip_prenorm=pre_done, drip=pending_post)
        elif pending_post:
            while pending_post:
                pending_post.pop(0)()
        pending_post = None
        if ci + 1 < nch:
            xload(ci + 1)
        pre_done = False
        if "mix" in stages:
            mixer(Xb, ci)

        def store(ci=ci, Xb=Xb):
            tr.add("sp", lambda e: e.dma_start(out=yh[ci], in_=Xb.ap.rearrange("p a b -> p (a b)")),
                   [Xb.r()], [], dma="D_y%d" % (ci % 2))
        if "ffn2" in stages:
            hook = None
            last = not (ci + 1 < nch and "ffn1" in stages and CUT > 3)
            if not last:
                hook = (lambda ci=ci: prenorm(X[(ci + 1) % 2], 0, bank=3))
                pre_done = True
            r_ = ffn(Xb, "f2", 4, 5, mid_hook=hook, defer_post=not last)
            if r_ is not None:
                pending_post = r_ + [store]
            else:
                store()
        else:
            store()
    final_waits = [("D_y0", tr.dma_count.get("D_y0", 0)), ("D_y1", tr.dma_count.get("D_y1", 0))]

    tr.finalize()
    sem_names = ["E_" + e for e in Tracker.ENGS] + sorted(tr.dma_count.keys())
    sems = {}
    import contextlib
    with contextlib.ExitStack() as es_:
        for n in sem_names:
            sems[n] = es_.enter_context(nc.semaphore(n))
        block = es_.enter_context(nc.Block())

        @block.sync
        def _(e):
            tr.emit("sp", e, sems)
            for n, v in final_waits:
                if v > 0:
                    e.wait_ge(sems[n], v)

        @block.gpsimd
        def _(e):
            tr.emit("pool", e, sems)

        @block.tensor
        def _(e):
            tr.emit("pe", e, sems)

        @block.scalar
        def _(e):
            tr.emit("act", e, sems)

        @block.vector
        def _(e):
            tr.emit("dve", e, sems)
    n_ops = {e: len(tr.ops[e]) for e in Tracker.ENGS}
    print("ops per engine:", n_ops)
    return nc


def _bucket(dist):
    n = np.clip(dist, 0, 127)
    max_exact = 16
    nf = np.maximum(n, 1).astype(np.float32)
    large = max_exact + (np.log(nf / np.float32(max_exact)) / np.float32(math.log(128 / max_exact))
                         * np.float32(32 - max_exact)).astype(np.int32)
    large = np.minimum(large, 31)
    return np.where(n < max_exact, n, large)


def pack_weights(inp):
    tiles = np.zeros((NPT, 128, TILE), np.float32)
    names = [n for n, _ in PASS_TILES]

    def put(name, arr):
        a = np.ascontiguousarray(arr).reshape(128, -1)
        tiles[names.index(name), :, :a.shape[1]] = a

    for pfx, key in (("f1", "ffn1"), ("f2", "ffn2")):
        Wg = inp[key + "_w_gate"][0]
        Wu = inp[key + "_w_up"][0]
        Wd = inp[key + "_w_down"][0]
        Wg_r = Wg.reshape(8, 128, NJ, 128).transpose(2, 1, 0, 3)
        Wu_r = Wu.reshape(8, 128, NJ, 128).transpose(2, 1, 0, 3)
        GU = np.stack([Wg_r, Wu_r], axis=2)
        for i in range(11):
            t_ = GU[2 * i:2 * i + 2].transpose(1, 0, 2, 3, 4)
            put(pfx + "gu%d" % i, t_)
        Wd_r = Wd.reshape(NJ, 128, 2, 512)
        for half in range(2):
            for ti in range(3):
                js = list(range(ti * 8, min(NJ, ti * 8 + 8)))
                t_ = Wd_r[js, :, half, :].transpose(1, 0, 2)
                put(pfx + "d%d" % (half * 3 + ti), t_)
    w_in = inp["w_in"][0]
    qperm = np.array([(half * 4 + c) * 64 + d for c in range(4) for half in range(2) for d in range(64)])

    def kmaj(cols):
        return cols.reshape(8, 128, -1).transpose(1, 0, 2)

    put("a0", kmaj(w_in[:, 0:512][:, qperm]))
    put("a1", kmaj(np.concatenate([w_in[:, 512:640], w_in[:, 640:768], w_in[:, 768:1024]], axis=1)))
    put("a2", kmaj(w_in[:, 1280:1792]))
    put("a3", kmaj(w_in[:, 1792:2304]))
    a4 = np.zeros((1024, 512), np.float32)
    a4[:, 0:256] = w_in[:, 1024:1280]
    a4[:, 256:272] = w_in[:, 2304:2320]
    put("a4", kmaj(a4))
    wpa = inp["w_proj_a"][0][qperm, :]
    wpb = inp["w_proj_b"][0]
    wga = w_in[:, 2320:3344]
    wgb = w_in[:, 3344:4368]
    for m in range(8):
        cs = slice(m * 128, (m + 1) * 128)
        parts = [wpa[:, cs].reshape(4, 128, 128).transpose(1, 0, 2).reshape(128, -1),
                 wpb[:, cs].reshape(4, 128, 128).transpose(1, 0, 2).reshape(128, -1),
                 wga[:, cs].reshape(8, 128, 128).transpose(1, 0, 2).reshape(128, -1),
                 wgb[:, cs].reshape(8, 128, 128).transpose(1, 0, 2).reshape(128, -1)]
        put("m%d" % m, np.concatenate(parts, axis=1))
    wo = inp["w_out"][0]
    for half in range(2):
        put("o%d" % half, kmaj(wo[:, half * 512:(half + 1) * 512]))
    return tiles


def pack_consts(inp):
    c = np.zeros((128, C_TOT), np.float32)
    gl = [inp["ffn1_pre_g"][0], inp["ffn1_post_g"][0], inp["mix_pre_g"][0], inp["mix_post_g"][0],
          inp["ffn2_pre_g"][0], inp["ffn2_post_g"][0]]
    for n, g in enumerate(gl):
        c[:, C_GAIN + n * 8:C_GAIN + (n + 1) * 8] = g.reshape(8, 128).T
    c[:, C_GLAG] = inp["gla_norm_g"][0]
    c[:, C_SINK:C_SINK + 8] = np.broadcast_to(inp["attn_sinks"][0][None, :], (128, 8))
    t_idx = np.arange(128)[:, None]
    j_idx = np.arange(256)[None, :]
    dist = t_idx + 128 - j_idx
    bk = _bucket(dist)
    bias = inp["rel_bias"][bk]
    c[:, C_BIAS:C_BIAS + 2048] = bias.transpose(0, 2, 1).reshape(128, 2048)
    valid = (dist >= 0) & (dist < 128)
    c[:, C_MASK:C_MASK + 256] = np.where(valid, 0.0, -1e30).astype(np.float32)
    c[:, C_IDENT:C_IDENT + 128] = np.eye(128, dtype=np.float32)
    s_i = np.arange(128)[:, None]
    t_i = np.arange(128)[None, :]
    same = (s_i // 64) == (t_i // 64)
    c[:, C_TRI:C_TRI + 128] = np.where(same & (s_i <= t_i), -1.0 / 16.0, 0.0)
    c[:, C_CMASK:C_CMASK + 128] = np.where(same & (s_i <= t_i), 1.0, 0.0)
    c[0:16, C_WAUG:C_WAUG + 256] = inp["w_alpha"][0]
    c[16, C_WAUG:C_WAUG + 256] = inp["b_alpha"][0]
    c[:, C_EPS] = EPS
    return c


_CACHE = {}


def kernel(**inputs):
    inp = {k: np.asarray(v) for k, v in inputs.items()}
    stages = tuple(os.environ.get("MK_STAGES", "ffn1,mix,ffn2").split(","))
    x = inp["x"]
    xr = x.reshape(NCORES, 2, 4, T, 8, 128)
    xh = np.ascontiguousarray(xr.transpose(0, 1, 2, 5, 4, 3)).reshape(NCORES, NCH, 128, 8 * T)
    wt = pack_weights(inp)
    cs = pack_consts(inp)
    nc = build_program(stages)
    in_maps = [{"xh": xh[i], "wst": wt, "cst": cs} for i in range(NCORES)]
    res = run_bass_kernel_spmd(nc, in_maps, core_ids=list(range(NCORES)))
    yh = np.stack([np.asarray(r["yh"]) for r in res.results], axis=0)
    y = yh.reshape(NCORES, 2, 4, 128, 8, T).transpose(0, 1, 2, 5, 4, 3).reshape(16, 2048, 1024)
    return np.ascontiguousarray(y.astype(np.float32))
```

```python
import os
import math
import numpy as np
import concourse.bass as bass
import concourse.mybir as mybir
from concourse.bass_utils import run_bass_kernel_spmd

F32 = mybir.dt.float32
BF16 = mybir.dt.bfloat16
AF = mybir.ActivationFunctionType
ALU = mybir.AluOpType
AX = mybir.AxisListType

D = 1024
DFF = 2816
NJ = 22
T = 512
NTT = 4
NCH = 8
NCORES = 8
EPS = 1e-6
RING = 5
TILE = 4096

GU_T = [("gu%d" % i, 4096) for i in range(11)]
D_T = [("d%d" % i, 4096) for i in range(6)]
MIX_T = [("a0", 4096), ("a1", 4096), ("a2", 4096), ("a3", 4096), ("a4", 8 * 512)] + \
        [("m%d" % m, 3072) for m in range(8)] + [("o0", 4096), ("o1", 4096)]
PASS_TILES = [("f1" + n, u) for n, u in GU_T + D_T] + MIX_T + [("f2" + n, u) for n, u in GU_T + D_T]
NPT = len(PASS_TILES)

C_GAIN = 0
C_GLAG = 48
C_SINK = 49
C_BIAS = 57
C_MASK = C_BIAS + 2048
C_IDENT = C_MASK + 256
C_TRI = C_IDENT + 128
C_CMASK = C_TRI + 128
C_WAUG = C_CMASK + 128
C_EPS = C_WAUG + 256
C_TOT = C_EPS + 1


class Op:
    __slots__ = ("eng", "fn", "deps", "signal", "sigval", "dma", "idx")

    def __init__(self, eng, fn, dma=None):
        self.eng = eng
        self.fn = fn
        self.deps = []
        self.signal = False
        self.sigval = 0
        self.dma = dma


class Tracker:
    ENGS = ("pe", "act", "dve", "pool", "sp")

    def __init__(self):
        self.ops = {e: [] for e in self.ENGS}
        self.segs = {}
        self.dma_count = {}

    def _dep(self, cons, prod, kind):
        if prod is None or prod is cons:
            return
        if prod.dma is None:
            if prod.eng == cons.eng:
                if prod.eng == "pe":
                    return
            prod.signal = True
        cons.deps.append(prod)

    def _touch(self, op, space, lo, hi, write):
        segs = self.segs.setdefault(space, [])
        new = []
        cur = lo
        out = []
        for s in segs:
            if s[1] <= lo or s[0] >= hi:
                out.append(s)
                continue
            if s[0] < lo:
                out.append([s[0], lo, s[2], dict(s[3]), list(s[4])])
                s = [lo, s[1], s[2], s[3], s[4]]
            if s[1] > hi:
                out.append([hi, s[1], s[2], dict(s[3]), list(s[4])])
                s = [s[0], hi, s[2], s[3], s[4]]
            new.append(s)
        new.sort(key=lambda s: s[0])
        filled = []
        for s in new:
            if s[0] > cur:
                filled.append([cur, s[0], None, {}, []])
            filled.append(s)
            cur = s[1]
        if cur < hi:
            filled.append([cur, hi, None, {}, []])
        for s in filled:
            if write:
                self._dep(op, s[2], "waw")
                for r in s[3].values():
                    self._dep(op, r, "war")
                for r in s[4]:
                    self._dep(op, r, "war")
                s[2] = op
                s[3] = {}
                s[4] = []
            else:
                self._dep(op, s[2], "raw")
                if op.dma is not None:
                    s[4].append(op)
                else:
                    s[3][op.eng] = op
        if write:
            filled = [[lo, hi, op, {}, []]]
        out.extend(filled)
        self.segs[space] = out

    def add(self, eng, fn, reads=(), writes=(), dma=None):
        d = None
        if dma is not None:
            self.dma_count[dma] = self.dma_count.get(dma, 0) + 16
            d = [dma, self.dma_count[dma]]
        op = Op(eng, fn, d)
        for (sp, lo, hi) in writes:
            if sp == "ps":
                self._touch(op, sp, lo // 2048 * 2048, (hi + 2047) // 2048 * 2048, True)
            else:
                self._touch(op, sp, lo, hi, True)
        for (sp, lo, hi) in reads:
            if sp == "ps":
                self._touch(op, sp, lo // 2048 * 2048, (hi + 2047) // 2048 * 2048, True)
            else:
                self._touch(op, sp, lo, hi, False)
        self.ops[eng].append(op)
        return op

    def finalize(self):
        for e in self.ENGS:
            k = 0
            for op in self.ops[e]:
                if op.signal and op.dma is None:
                    k += 1
                    op.sigval = k

    def emit(self, eng_name, eng, sems):
        waited = {}
        for op in self.ops[eng_name]:
            need = {}
            for p in op.deps:
                if p.dma is not None:
                    key, val = p.dma[0], p.dma[1]
                else:
                    key, val = "E_" + p.eng, p.sigval
                if val > need.get(key, 0):
                    need[key] = val
            for key, val in need.items():
                if waited.get(key, 0) < val:
                    eng.wait_ge(sems[key], val)
                    waited[key] = val
            ins = op.fn(eng)
            if op.dma is not None:
                ins.then_inc(sems[op.dma[0]], 16)
            elif op.signal:
                ins.then_inc(sems["E_" + eng_name], 1)


class Buf:
    def __init__(self, space, base, off_bytes, dtype, shape, npart=128):
        self.space = space
        self.off = off_bytes
        self.es = 2 if dtype == BF16 else 4
        self.shape = tuple(shape)
        n = int(np.prod(shape))
        self.n = n
        w0 = off_bytes // 4
        w1 = (off_bytes + n * self.es + 3) // 4
        v = base[:, w0:w1]
        if dtype != F32:
            v = v.bitcast(dtype)
        if len(shape) == 2:
            v = v.rearrange("p (a b) -> p a b", a=shape[0])
        elif len(shape) == 3:
            v = v.rearrange("p (a b c) -> p a b c", a=shape[0], b=shape[1])
        self.ap = v

    def r(self, *idx, lo=None, hi=None):
        stride = self.n
        off = 0
        for i, ix in enumerate(idx):
            stride //= self.shape[i]
            off += ix * stride
        a = 0 if lo is None else lo
        b = stride if hi is None else hi
        return (self.space, self.off + (off + a) * self.es, self.off + (off + b) * self.es)


def build_program(stages=("ffn1", "mix", "ffn2"), nch=NCH):
    nc = bass.Bass("TRN2", target_bir_lowering=False)
    xh = nc.dram_tensor("xh", [NCH, 128, 8 * T], F32, kind="ExternalInput").ap()
    wst = nc.dram_tensor("wst", [NPT, 128, TILE], F32, kind="ExternalInput").ap()
    cst = nc.dram_tensor("cst", [128, C_TOT], F32, kind="ExternalInput").ap()
    yh = nc.dram_tensor("yh", [NCH, 128, 8 * T], F32, kind="ExternalOutput").ap()

    SBW = 53000
    sb = nc.alloc_sbuf_tensor("sb", [128, SBW], F32)
    ps = nc.alloc_psum_tensor("ps", [128, 4096], F32)
    tr = Tracker()
    alloc = {"off": 0}

    def sbuf(dtype, shape):
        es = 2 if dtype == BF16 else 4
        n = int(np.prod(shape)) * es
        n = (n + 63) // 64 * 64
        b = Buf("sb", sb, alloc["off"], dtype, shape)
        alloc["off"] += n
        assert alloc["off"] <= SBW * 4, alloc["off"]
        return b

    def psum(bank, off_words, dtype, shape):
        return Buf("ps", ps, (bank * 512 + off_words) * 4, dtype, shape)

    def region(nbytes):
        o = alloc["off"]
        alloc["off"] += (nbytes + 63) // 64 * 64
        assert alloc["off"] <= SBW * 4, alloc["off"]
        return {"base": o, "off": o, "end": o + nbytes}

    def rsub(reg, dtype, shape):
        es = 2 if dtype == BF16 else 4
        n = (int(np.prod(shape)) * es + 63) // 64 * 64
        b = Buf("sb", sb, reg["off"], dtype, shape)
        reg["off"] += n
        assert reg["off"] <= reg["end"], (reg, n)
        return b

    def rreset(reg):
        reg["off"] = reg["base"]

    X = [sbuf(F32, (8, T)) for _ in range(2)]
    hT = sbuf(BF16, (8, T))
    sq = sbuf(BF16, (8, T))
    rstd = sbuf(F32, (T,))
    ring = [sbuf(BF16, (TILE,)) for _ in range(RING)]
    CST = sbuf(F32, (C_TOT,))
    ident = sbuf(BF16, (128,))
    onesA = sbuf(BF16, (128,))
    onesB = sbuf(BF16, (128,))
    nsink = sbuf(F32, (8,))
    gs = [sbuf(BF16, (T,)) for _ in range(2)]
    agc = sbuf(F32, (3, 8))
    dummy = sbuf(F32, (2,))
    kaT = sbuf(BF16, (128 + T,))
    va = sbuf(BF16, (5, 128))
    alrT = sbuf(F32, (T,))
    yaT = sbuf(BF16, (4, T))
    ybT = sbuf(BF16, (4, T))
    pT = [sbuf(BF16, (8, 128)) for _ in range(2)]
    st_mx = [sbuf(F32, (4,)) for _ in range(2)]
    st_negm = [sbuf(F32, (4,)) for _ in range(2)]
    st_rsum = [sbuf(F32, (4,)) for _ in range(2)]
    st_t = [sbuf(F32, (4,)) for _ in range(2)]
    st_es = [sbuf(F32, (4,)) for _ in range(2)]
    st_rden = [sbuf(F32, (4,)) for _ in range(2)]
    e1 = sbuf(F32, (256,))
    spb = sbuf(F32, (256,))
    eb = sbuf(F32, (2, 128))
    enb = sbuf(F32, (2, 128))
    eks = sbuf(F32, (2, 128))
    blast = sbuf(F32, (2, 2))
    decay = sbuf(F32, (2, 2))
    attnT = [sbuf(BF16, (4, 128)) for _ in range(2)]
    S = sbuf(F32, (2, 128))
    Sbf = sbuf(BF16, (8, 2, 128))
    regA = region(NJ * T * 2)
    actT = rsub(regA, BF16, (NJ, T))
    rreset(regA)
    qaT = rsub(regA, BF16, (4, T))
    qbT = rsub(regA, F32, (2, T))
    kbT = rsub(regA, F32, (2, T))
    vb = rsub(regA, BF16, (4, 512))
    rbT = rsub(regA, BF16, (4, T))
    regF = region(8 * T * 4)
    fT = rsub(regF, F32, (8, T))
    rreset(regF)
    oT = rsub(regF, F32, (4, T))
    rstd4 = rsub(regF, F32, (4, T))
    regS = region(16384)
    s_sb = [rsub(regS, F32, (4, 256)) for _ in range(2)]
    p_sb = [rsub(regS, BF16, (4, 256)) for _ in range(2)]
    pn_sb = [rsub(regS, BF16, (4, 256)) for _ in range(2)]
    rreset(regS)
    sga = [rsub(regS, F32, (T,)) for _ in range(2)]
    sgb = [rsub(regS, F32, (T,)) for _ in range(2)]
    t1 = [rsub(regS, F32, (T,)) for _ in range(2)]
    t2 = [rsub(regS, F32, (T,)) for _ in range(2)]
    rreset(regS)
    spb4 = [rsub(regS, F32, (256,)) for _ in range(4)]
    eb4 = [rsub(regS, F32, (2, 128)) for _ in range(4)]
    enb4 = [rsub(regS, F32, (2, 128)) for _ in range(4)]
    eks4 = [rsub(regS, F32, (2, 128)) for _ in range(4)]
    attnT4 = attnT + [sbuf(BF16, (4, 128)) for _ in range(2)]
    blast4 = [sbuf(F32, (2, 2)) for _ in range(4)]
    decay4 = [sbuf(F32, (2, 2)) for _ in range(4)]
    regM = region(8 * T * 2)
    mgT = rsub(regM, BF16, (8, T))
    rreset(regM)
    qdec = rsub(regM, BF16, (2, T))
    kin = rsub(regM, BF16, (2, T))
    kst = rsub(regM, BF16, (2, T))
    kst_tok = rsub(regM, BF16, (4, 256))
    print("SBUF bytes/partition used:", alloc["off"])

    def mm(out_ap, lhsT, rhs, start, stop, reads, writes):
        tr.add("pe", lambda e: e.matmul(out_ap, lhsT, rhs, start=start, stop=stop), reads, writes)

    def tp(out_ap, in_ap, reads, writes):
        tr.add("pe", lambda e: e.transpose(out_ap, in_ap, ident.ap[:, :]), reads + [ident.r()], writes)

    def act(out_ap, in_ap, func, reads, writes, bias=None, scale=None, accum=None):
        kw = {}
        if bias is not None:
            kw["bias"] = bias
        if scale is not None:
            kw["scale"] = scale
        if accum is not None:
            kw["accum_out"] = accum
        tr.add("act", lambda e: e.activation(out_ap, in_ap, func, **kw), reads, writes)

    def dve(fn, reads, writes):
        tr.add("dve", fn, reads, writes)

    tr.add("sp", lambda e: e.dma_start(out=CST.ap, in_=cst), [], [CST.r()], dma="D_cst")
    cview = CST.ap
    dve(lambda e: e.tensor_copy(out=ident.ap, in_=cview[:, C_IDENT:C_IDENT + 128]),
        [CST.r(lo=C_IDENT, hi=C_IDENT + 128)], [ident.r()])
    dve(lambda e: e.memset(onesA.ap, 1.0 / 1024.0), [], [onesA.r()])
    dve(lambda e: e.memset(onesB.ap, 1.0 / 128.0), [], [onesB.r()])
    dve(lambda e: e.tensor_scalar(out=nsink.ap, in0=cview[:, C_SINK:C_SINK + 8], scalar1=-1.0, scalar2=None,
                                  op0=ALU.mult),
        [CST.r(lo=C_SINK, hi=C_SINK + 8)], [nsink.r()])
    biasv = cview[:, C_BIAS:C_BIAS + 2048].rearrange("p (h s) -> p h s", h=8)
    maskv = cview[:, C_MASK:C_MASK + 256]
    dve(lambda e: e.tensor_tensor(out=biasv, in0=biasv, in1=maskv.unsqueeze(1).to_broadcast([128, 8, 256]),
                                  op=ALU.add),
        [CST.r(lo=C_BIAS, hi=C_MASK + 256)], [CST.r(lo=C_BIAS, hi=C_BIAS + 2048)])
    dve(lambda e: e.memset(alrT.ap[0:32, :], 1.0), [], [alrT.r()])
    for i_, (gi_, al_) in enumerate(((1, 0.5), (3, 1.0), (5, 0.5))):
        dve(lambda e, i_=i_, gi_=gi_, al_=al_: e.tensor_scalar(
            out=agc.ap[:, i_, :], in0=cview[:, C_GAIN + gi_ * 8:C_GAIN + gi_ * 8 + 8], scalar1=float(al_), scalar2=None,
            op0=ALU.mult), [CST.r(lo=0, hi=57)], [agc.r(i_)])
    AGI = {1: 0, 3: 1, 5: 2}
    triv = cview[:, C_TRI:C_TRI + 128]
    cmaskv = cview[:, C_CMASK:C_CMASK + 128]
    waugv = cview[0:32, C_WAUG:C_WAUG + 256]
    R_TRI = CST.r(lo=C_TRI, hi=C_TRI + 128)
    R_CMASK = CST.r(lo=C_CMASK, hi=C_CMASK + 128)
    R_WAUG = CST.r(lo=C_WAUG, hi=C_WAUG + 256)
    R_EPS = CST.r(lo=C_EPS, hi=C_EPS + 1)
    epsv = cview[:, C_EPS:C_EPS + 1]

    def gain(n, c):
        return cview[:, C_GAIN + n * 8 + c:C_GAIN + n * 8 + c + 1]

    R_GAIN = CST.r(lo=0, hi=57)

    wstate = {"g": 0}

    def wload(pidx):
        g = wstate["g"]
        wstate["g"] += 1
        slot = g % RING
        used = PASS_TILES[pidx][1]
        b = ring[slot]
        tr.add("pool", lambda e: e.dma_start(out=b.ap[:, 0:used], in_=wst[pidx][:, 0:used]),
               [], [b.r()], dma="D_w%d" % slot)
        return b

    pending = []
    CUT_ = int(os.environ.get("MK_CUT", "99"))
    order = {"pos": 0, "seq": []}
    for ci in range(nch):
        for pidx in range(NPT):
            nm = PASS_TILES[pidx][0]
            if nm.startswith("f1") and "ffn1" not in stages:
                continue
            if nm.startswith("f2") and "ffn2" not in stages:
                continue
            if not (nm.startswith("f1") or nm.startswith("f2")) and "mix" not in stages:
                continue
            if nm[:2] in ("f1", "f2"):
                if CUT_ <= 1:
                    continue
                if CUT_ <= 2 and nm[2] == "d":
                    continue
            order["seq"].append(pidx)

    def wnext(expect_name):
        while len(pending) < RING and order["pos"] < len(order["seq"]):
            pidx = order["seq"][order["pos"]]
            order["pos"] += 1
            pending.append((pidx, wload(pidx)))
        pidx, b = pending.pop(0)
        assert PASS_TILES[pidx][0] == expect_name, (PASS_TILES[pidx][0], expect_name)
        return b

    def wrefill():
        while len(pending) < RING - 1 and order["pos"] < len(order["seq"]):
            pidx = order["seq"][order["pos"]]
            order["pos"] += 1
            pending.append((pidx, wload(pidx)))

    def rms_stats(bank):
        mps = psum(bank, 0, F32, (T,))
        for c in range(8):
            mm(mps.ap, onesA.ap, sq.ap[:, c, :], c == 0, c == 7,
               [onesA.r(), sq.r(c)], [mps.r()])
        act(rstd.ap, mps.ap, AF.Ln, [mps.r(), R_EPS], [rstd.r()], bias=epsv)
        act(rstd.ap, rstd.ap, AF.Exp, [rstd.r()], [rstd.r()], scale=-0.5)

    def prenorm(Xb, gidx, bank=0):
        for c in range(8):
            act(sq.ap[:, c, :], Xb.ap[:, c, :], AF.Square, [Xb.r(c)], [sq.r(c)])
        rms_stats(bank)
        for c in range(8):
            dve(lambda e, c=c: e.scalar_tensor_tensor(out=hT.ap[:, c, :], in0=Xb.ap[:, c, :], scalar=gain(gidx, c),
                                                      in1=rstd.ap, op0=ALU.mult, op1=ALU.mult),
                [Xb.r(c), R_GAIN, rstd.r()], [hT.r(c)])

    def postnorm(Xb, gidx, alpha, fbanks):
        for m in range(8):
            fp = fbanks[m]
            act(sq.ap[:, m, :], fp.ap, AF.Square, [fp.r()], [sq.r(m)])
            dve(lambda e, m=m, fp=fp: e.tensor_copy(out=fT.ap[:, m, :], in_=fp.ap), [fp.r()], [fT.r(m)])

    def postnorm_ops(Xb, bank):
        def stats():
            mps = psum(bank, 0, F32, (T,))
            for c in range(8):
                mm(mps.ap, onesA.ap, sq.ap[:, c, :], c == 0, c == 7, [onesA.r(), sq.r(c)], [mps.r()])
            act(rstd.ap, mps.ap, AF.Ln, [mps.r(), R_EPS], [rstd.r()], bias=epsv)
        ops = [stats,
               lambda: act(rstd.ap, rstd.ap, AF.Exp, [rstd.r()], [rstd.r()], scale=-0.5)]
        def mul_(m):
            return lambda: dve(lambda e: e.tensor_tensor(out=fT.ap[:, m, :], in0=fT.ap[:, m, :], in1=rstd.ap,
                                                         op=ALU.mult), [fT.r(m), rstd.r()], [fT.r(m)])

        def add_(m):
            return lambda: dve(lambda e: e.tensor_tensor(out=Xb.ap[:, m, :], in0=Xb.ap[:, m, :], in1=fT.ap[:, m, :],
                                                         op=ALU.add), [fT.r(m), Xb.r(m)], [Xb.r(m)])
        ops.append(mul_(0))
        for m in range(1, 8):
            ops.append(mul_(m))
            ops.append(add_(m - 1))
        ops.append(add_(7))
        return ops

    def postnorm_finish(Xb, gidx, alpha, bank=0):
        for f_ in postnorm_ops(Xb, bank):
            f_()

    CUT = int(os.environ.get("MK_CUT", "99"))

    def ffn(Xb, pfx, g_pre, g_post, skip_prenorm=False, mid_hook=None, drip=None, defer_post=False):
        if CUT <= 0:
            return
        if not skip_prenorm:
            prenorm(Xb, g_pre)
        if CUT <= 1:
            return
        gps = [psum(0, 0, F32, (T,)), psum(1, 0, F32, (T,))]
        ups = [psum(2, 0, F32, (T,)), psum(3, 0, F32, (T,))]
        wt = None
        for j in range(NJ):
            if j % 2 == 0:
                wt = wnext(pfx + "gu%d" % (j // 2))
                wv = wt.ap.rearrange("p (jj gu k c) -> p jj gu k c", jj=2, gu=2, k=8)
            jj = j % 2
            gp, up = gps[j % 2], ups[j % 2]
            if j == 0:
                for k in range(8):
                    for j2 in range(2):
                        mm(gps[j2].ap, wv[:, j2, 0, k, :], hT.ap[:, k, :], k == 0, k == 7, [wt.r(), hT.r(k)], [gps[j2].r()])
                        mm(ups[j2].ap, wv[:, j2, 1, k, :], hT.ap[:, k, :], k == 0, k == 7, [wt.r(), hT.r(k)], [ups[j2].r()])
            elif j >= 2:
                for k in range(8):
                    mm(gp.ap, wv[:, jj, 0, k, :], hT.ap[:, k, :], k == 0, k == 7, [wt.r(), hT.r(k)], [gp.r()])
                for k in range(8):
                    mm(up.ap, wv[:, jj, 1, k, :], hT.ap[:, k, :], k == 0, k == 7, [wt.r(), hT.r(k)], [up.r()])
            g_ = gs[j % 2]
            act(g_.ap, gp.ap, AF.Silu, [gp.r()], [g_.r()])
            dve(lambda e, j=j, g_=g_, up=up: e.tensor_tensor(out=actT.ap[:, j, :], in0=g_.ap, in1=up.ap, op=ALU.mult),
                [g_.r(), up.r()], [actT.r(j)])
            if drip:
                for _ in range(3 if j > 0 else 1):
                    if drip:
                        drip.pop(0)()
            wrefill()
        while drip:
            drip.pop(0)()
        if CUT <= 2:
            return
        if mid_hook is not None:
            mid_hook()
        else:
            act(dummy.ap[:, 0:1], epsv, AF.Ln, [R_EPS], [dummy.r()])
        for half in range(2):
            fps = [psum((4 if half == 0 else 0) + i, 0, F32, (T,)) for i in range(4)]
            for ti in range(3):
                wt = wnext(pfx + "d%d" % (half * 3 + ti))
                wv = wt.ap.rearrange("p (jj n) -> p jj n", jj=8)
                for jj in range(8):
                    j = ti * 8 + jj
                    if j >= NJ:
                        break
                    for i in range(4):
                        mm(fps[i].ap, wv[:, jj, i * 128:(i + 1) * 128], actT.ap[:, j, :], j == 0, j == NJ - 1,
                           [wt.r(), actT.r(j)], [fps[i].r()])
                wrefill()
            for i in range(4):
                m = half * 4 + i
                fp = fps[i]
                act(sq.ap[:, m, :], fp.ap, AF.Square, [fp.r()], [sq.r(m)])
                dve(lambda e, m=m, fp=fp: e.tensor_scalar(out=fT.ap[:, m, :], in0=fp.ap,
                                                          scalar1=agc.ap[:, AGI[g_post], m:m + 1], scalar2=None, op0=ALU.mult),
                    [fp.r(), agc.r()], [fT.r(m)])
        if CUT <= 3:
            return
        if defer_post:
            ops_ = postnorm_ops(Xb, 4)
            ops_.pop(0)()
            return ops_
        postnorm_finish(Xb, g_post, 0.5, bank=4)
        return None

    def proj_fm(wt, wv, col0, ncols, bank, evac):
        pb = psum(bank, 0, F32, (T,))
        for k in range(8):
            mm(pb.ap[0:ncols, :], wv[:, k, col0:col0 + ncols], hT.ap[:, k, :], k == 0, k == 7,
               [wt.r(), hT.r(k)], [pb.r()])
        evac(pb)

    def mixer(Xb, ci):
        first_of_seq = (ci % 4 == 0)
        prenorm(Xb, 2)
        bank = {"i": 0}

        def nb():
            b = bank["i"]
            bank["i"] = (b + 1) % 4
            return b

        wt = wnext("a0")
        wv = wt.ap.rearrange("p (k c) -> p k c", k=8)
        qps = [psum(c, 0, F32, (T,)) for c in range(4)]
        for k in range(8):
            for c in range(4):
                mm(qps[c].ap, wv[:, k, c * 128:(c + 1) * 128], hT.ap[:, k, :], k == 0, k == 7,
                   [wt.r(), hT.r(k)], [qps[c].r()])
        for c in range(4):
            act(qaT.ap[:, c, :], qps[c].ap, AF.Copy, [qps[c].r()], [qaT.r(c)])
        wrefill()
        wt = wnext("a1")
        wv = wt.ap.rearrange("p (k c) -> p k c", k=8)
        proj_fm(wt, wv, 0, 128, nb(),
                lambda pb: act(kaT.ap[:, 128:128 + T], pb.ap, AF.Copy, [pb.r()], [kaT.r(lo=128, hi=128 + T)]))
        pbv = psum(nb(), 0, F32, (4, 128))
        for tt in range(NTT):
            for k in range(8):
                mm(pbv.ap[:, tt, :], hT.ap[:, k, tt * 128:(tt + 1) * 128], wv[:, k, 128:256], k == 0, k == 7,
                   [wt.r(), hT.r(k)], [pbv.r(tt)])
        act(va.ap[:, 1:5, :], pbv.ap, AF.Copy, [pbv.r()], [va.r(lo=128, hi=640)])
        for c in range(2):
            proj_fm(wt, wv, 256 + c * 128, 128, nb(),
                    lambda pb, c=c: dve(lambda e: e.tensor_copy(out=qbT.ap[:, c, :], in_=pb.ap), [pb.r()], [qbT.r(c)]))
        wrefill()
        wt = wnext("a2")
        wv = wt.ap.rearrange("p (k c) -> p k c", k=8)
        for tt in range(NTT):
            pb = psum(nb(), 0, F32, (T,))
            for k in range(8):
                mm(pb.ap, hT.ap[:, k, tt * 128:(tt + 1) * 128], wv[:, k, :], k == 0, k == 7,
                   [wt.r(), hT.r(k)], [pb.r()])
            act(vb.ap[:, tt, :], pb.ap, AF.Copy, [pb.r()], [vb.r(tt)])
        wrefill()
        wt = wnext("a3")
        wv = wt.ap.rearrange("p (k c) -> p k c", k=8)
        for c in range(4):
            proj_fm(wt, wv, c * 128, 128, nb(),
                    lambda pb, c=c: act(rbT.ap[:, c, :], pb.ap, AF.Silu, [pb.r()], [rbT.r(c)]))
        wrefill()
        wt = wnext("a4")
        wv = wt.ap.rearrange("p (k c) -> p k c", k=8)
        for c in range(2):
            proj_fm(wt, wv, c * 128, 128, nb(),
                    lambda pb, c=c: dve(lambda e: e.tensor_copy(out=kbT.ap[:, c, :], in_=pb.ap), [pb.r()], [kbT.r(c)]))
        proj_fm(wt, wv, 256, 16, nb(),
                lambda pb: dve(lambda e: e.tensor_copy(out=alrT.ap[0:16, :], in_=pb.ap[0:16, :]), [pb.r()], [alrT.r()]))
        wrefill()

        H2 = (0, 1)
        spsl = [psum(0, 0, F32, (4, 256)), psum(2, 0, F32, (4, 256))]
        tpsl = [psum(4, 0, BF16, (8, 128)), psum(5, 0, BF16, (8, 128))]
        hsl = [slice(0, 64), slice(64, 128)]
        yps = psum(6, 0, F32, (4, 128))

        def blk_params(tt):
            first_blk = first_of_seq and tt == 0
            ncol = 128 if first_blk else 256
            boff = 128 if first_blk else 0
            kc0 = tt * 128 + boff
            blks = [1] if first_blk else [0, 1]
            return first_blk, ncol, boff, kc0, blks

        def swa_scores(tt):
            first_blk, ncol, boff, kc0, blks = blk_params(tt)
            for half in H2:
                for c in range(4):
                    mm(spsl[half].ap[:, c, 0:ncol], qaT.ap[hsl[half], c, tt * 128:(tt + 1) * 128],
                       kaT.ap[hsl[half], kc0:kc0 + ncol],
                       True, True, [qaT.r(c), kaT.r(lo=kc0, hi=kc0 + ncol)], [spsl[half].r(c)])

        def swa_chain_a(tt):
            first_blk, ncol, boff, kc0, blks = blk_params(tt)
            for half in H2:
                dve(lambda e, sps=spsl[half], s_=s_sb[half], half=half, ncol=ncol, boff=boff: e.scalar_tensor_tensor(
                    out=s_.ap[:, :, 0:ncol], in0=sps.ap[:, :, 0:ncol], scalar=0.125,
                    in1=biasv[:, half * 4:half * 4 + 4, boff:boff + ncol], op0=ALU.mult, op1=ALU.add),
                    [spsl[half].r(), CST.r(lo=C_BIAS, hi=C_BIAS + 2048)], [s_sb[half].r()])
                dve(lambda e, s_=s_sb[half], mx=st_mx[half], ncol=ncol: e.tensor_reduce(
                    out=mx.ap, in_=s_.ap[:, :, 0:ncol], axis=AX.X, op=ALU.max),
                    [s_sb[half].r()], [st_mx[half].r()])
                dve(lambda e, mx=st_mx[half], negm=st_negm[half], half=half: e.scalar_tensor_tensor(
                    out=negm.ap, in0=mx.ap, scalar=-1.0, in1=nsink.ap[:, half * 4:half * 4 + 4],
                    op0=ALU.mult, op1=ALU.min),
                    [st_mx[half].r(), nsink.r()], [st_negm[half].r()])
                dve(lambda e, tt_=st_t[half], negm=st_negm[half], half=half: e.tensor_tensor(
                    out=tt_.ap, in0=negm.ap, in1=cview[:, C_SINK + half * 4:C_SINK + half * 4 + 4], op=ALU.add),
                    [st_negm[half].r(), CST.r(lo=C_SINK, hi=C_SINK + 8)], [st_t[half].r()])
                p_, s_, negm, rsum = p_sb[half], s_sb[half], st_negm[half], st_rsum[half]
                for c in range(4):
                    act(p_.ap[:, c, 0:ncol], s_.ap[:, c, 0:ncol], AF.Exp, [s_.r(c), negm.r()], [p_.r(c), rsum.r()],
                        bias=negm.ap[:, c:c + 1], accum=rsum.ap[:, c:c + 1])
                act(st_es[half].ap, st_t[half].ap, AF.Exp, [st_t[half].r()], [st_es[half].r()])

        def swa_chain_b(tt):
            first_blk, ncol, boff, kc0, blks = blk_params(tt)
            for half in H2:
                dve(lambda e, es=st_es[half], rsum=st_rsum[half]: e.tensor_tensor(out=es.ap, in0=es.ap, in1=rsum.ap, op=ALU.add),
                    [st_es[half].r(), st_rsum[half].r()], [st_es[half].r()])
                dve(lambda e, es=st_es[half], rden=st_rden[half]: e.reciprocal(out=rden.ap, in_=es.ap),
                    [st_es[half].r()], [st_rden[half].r()])
                p_, pn_, rden = p_sb[half], pn_sb[half], st_rden[half]
                for c in range(4):
                    dve(lambda e, p_=p_, pn_=pn_, rden=rden, c=c, ncol=ncol: e.tensor_scalar(
                        out=pn_.ap[:, c, 0:ncol], in0=p_.ap[:, c, 0:ncol], scalar1=rden.ap[:, c:c + 1], scalar2=None,
                        op0=ALU.mult),
                        [p_.r(c), rden.r()], [pn_.r(c)])
            for half in H2:
                tps, pn_ = tpsl[half], pn_sb[half]
                for c in range(4):
                    for bi, blk in enumerate(blks):
                        tp(tps.ap[:, c * 2 + blk, :], pn_.ap[:, c, bi * 128:(bi + 1) * 128],
                           [pn_.r(c)], [tps.r(c * 2 + blk)])
            for half in H2:
                tps, pT_ = tpsl[half], pT[half]
                if first_blk:
                    for c in range(4):
                        dve(lambda e, pT_=pT_, tps=tps, c=c: e.tensor_copy(out=pT_.ap[:, c * 2 + 1, :], in_=tps.ap[:, c * 2 + 1, :]),
                            [tps.r(c * 2 + 1)], [pT_.r(c * 2 + 1)])
                else:
                    dve(lambda e, pT_=pT_, tps=tps: e.tensor_copy(out=pT_.ap, in_=tps.ap), [tps.r()], [pT_.r()])
            for half in H2:
                pT_ = pT[half]
                for c in range(4):
                    for bi, blk in enumerate(blks):
                        mm(yps.ap[hsl[half], c, :], va.ap[:, tt + blk, half * 64:(half + 1) * 64], pT_.ap[:, c * 2 + blk, :],
                           bi == 0, bi == len(blks) - 1,
                           [va.r(tt + blk), pT_.r(c * 2 + blk)], [yps.r(c)])
            act(yaT.ap[:, :, tt * 128:(tt + 1) * 128], yps.ap, AF.Copy, [yps.r()], [yaT.r()])

        swa_scores(0)
        swa_chain_a(0)
        for tt in range(NTT):
            if tt + 1 < NTT:
                swa_scores(tt + 1)
            swa_chain_b(tt)
            if tt + 1 < NTT:
                swa_chain_a(tt + 1)
        dve(lambda e: e.tensor_copy(out=kaT.ap[:, 0:128], in_=kaT.ap[:, T:T + 128]),
            [kaT.r(lo=T, hi=T + 128)], [kaT.r(lo=0, hi=128)])
        dve(lambda e: e.tensor_copy(out=va.ap[:, 0, :], in_=va.ap[:, 4, :]), [va.r(4)], [va.r(0)])

        if first_of_seq:
            dve(lambda e: e.memset(S.ap, 0.0), [], [S.r()])
        TCS = [slice(tt * 128, (tt + 1) * 128) for tt in range(NTT)]
        zps = [psum(tt, 0, F32, (256,)) for tt in range(NTT)]
        bps = [psum(tt, 256, F32, (2, 128)) for tt in range(NTT)]
        trps = [psum(6, tt * 128, BF16, (2, 128)) for tt in range(NTT)]
        dpsl = [psum(4, 0, F32, (2, 128)), psum(5, 0, F32, (2, 128))]
        apsl = [[psum(2 * (tt % 2) + half, 0 if tt < 2 else 256, F32, (2, 128)) for half in range(2)] for tt in range(NTT)]
        opsl = [[psum(4 + 2 * (tt % 2) + half, 0 if tt < 2 else 256, F32, (2, 128)) for half in range(2)] for tt in range(NTT)]
        for tt in range(NTT):
            mm(zps[tt].ap, alrT.ap[0:32, TCS[tt]], waugv, True, True, [alrT.r(), R_WAUG], [zps[tt].r()])
        for tt in range(NTT):
            act(spb4[tt].ap, zps[tt].ap, AF.Exp, [zps[tt].r()], [spb4[tt].r()], scale=-1.0)
            act(spb4[tt].ap, spb4[tt].ap, AF.Ln, [spb4[tt].r()], [spb4[tt].r()], bias=1.0)
        for tt in range(NTT):
            for fc in range(2):
                mm(bps[tt].ap[:, fc, :], spb4[tt].ap[:, fc * 128:(fc + 1) * 128], triv, True, True,
                   [spb4[tt].r(), R_TRI], [bps[tt].r(fc)])
        for tt in range(NTT):
            dve(lambda e, tt=tt: e.tensor_copy(out=blast4[tt].ap, in_=bps[tt].ap[:, :, 63:128:64]),
                [bps[tt].r()], [blast4[tt].r()])
        for tt in range(NTT):
            act(eb4[tt].ap, bps[tt].ap, AF.Exp, [bps[tt].r()], [eb4[tt].r()])
            act(enb4[tt].ap, bps[tt].ap, AF.Exp, [bps[tt].r()], [enb4[tt].r()], scale=-1.0)
            act(decay4[tt].ap, blast4[tt].ap, AF.Exp, [blast4[tt].r()], [decay4[tt].r()])
        for tt in range(NTT):
            tcs = TCS[tt]
            dve(lambda e, tcs=tcs, tt=tt: e.scalar_tensor_tensor(out=qdec.ap[:, :, tcs], in0=qbT.ap[:, :, tcs], scalar=0.125,
                                                                 in1=eb4[tt].ap, op0=ALU.mult, op1=ALU.mult),
                [qbT.r(), eb4[tt].r()], [qdec.r(0, lo=tt * 128, hi=tt * 128 + 128), qdec.r(1, lo=tt * 128, hi=tt * 128 + 128)])
            dve(lambda e, tcs=tcs, tt=tt: e.tensor_tensor(out=kin.ap[:, :, tcs], in0=kbT.ap[:, :, tcs], in1=enb4[tt].ap, op=ALU.mult),
                [kbT.r(), enb4[tt].r()], [kin.r(0, lo=tt * 128, hi=tt * 128 + 128), kin.r(1, lo=tt * 128, hi=tt * 128 + 128)])
            for fc in range(2):
                for sub in range(2):
                    cs_ = slice(tt * 128 + sub * 64, tt * 128 + (sub + 1) * 64)
                    dve(lambda e, tt=tt, fc=fc, sub=sub, cs_=cs_: e.scalar_tensor_tensor(
                        out=kst.ap[:, fc, cs_], in0=kbT.ap[:, fc, cs_], scalar=decay4[tt].ap[:, fc, sub:sub + 1],
                        in1=enb4[tt].ap[:, fc, sub * 64:(sub + 1) * 64], op0=ALU.mult, op1=ALU.mult),
                        [kbT.r(fc), decay4[tt].r(), enb4[tt].r(fc)],
                        [kst.r(fc, lo=tt * 128 + sub * 64, hi=tt * 128 + (sub + 1) * 64)])
        for tt in range(NTT):
            for fc in range(2):
                tp(trps[tt].ap[:, fc, :], kst.ap[:, fc, TCS[tt]], [kst.r(fc, lo=tt * 128, hi=tt * 128 + 128)], [trps[tt].r(fc)])
        for tt in range(NTT):
            act(kst_tok.ap[:, tt, :], trps[tt].ap.rearrange("p a b -> p (a b)"), AF.Copy, [trps[tt].r()], [kst_tok.r(tt)])
        for tt in range(NTT):
            for sub in range(2):
                sc = tt * 2 + sub
                dps = dpsl[sub]
                ss = slice(sub * 64, (sub + 1) * 64)
                for h in range(4):
                    fc, half = h // 2, h % 2
                    mm(dps.ap[half * 64:(half + 1) * 64, fc, :], kst_tok.ap[ss, tt, h * 64:(h + 1) * 64],
                       vb.ap[ss, tt, h * 128:(h + 1) * 128], True, True,
                       [kst_tok.r(tt), vb.r(tt)], [dps.r(fc)])
                dve(lambda e, sc=sc: e.tensor_copy(out=Sbf.ap[:, sc, :, :], in_=S.ap), [S.r()], [Sbf.r(sc)])
                for fc in range(2):
                    dve(lambda e, fc=fc, sub=sub, dps=dps, tt=tt: e.scalar_tensor_tensor(
                        out=S.ap[:, fc, :], in0=S.ap[:, fc, :], scalar=decay4[tt].ap[:, fc, sub:sub + 1],
                        in1=dps.ap[:, fc, :], op0=ALU.mult, op1=ALU.add),
                        [S.r(fc), decay4[tt].r(), dps.r(fc)], [S.r(fc)])
        for tt in range(NTT):
            tcs = TCS[tt]
            for h in range(4):
                fc, half = h // 2, h % 2
                hs = slice(half * 64, (half + 1) * 64)
                mm(apsl[tt][half].ap[:, fc, :], kin.ap[hs, fc, tcs], qdec.ap[hs, fc, tcs], True, True,
                   [kin.r(fc, lo=tt * 128, hi=tt * 128 + 128), qdec.r(fc, lo=tt * 128, hi=tt * 128 + 128)],
                   [apsl[tt][half].r(fc)])
        for tt in range(NTT):
            at_ = attnT4[tt]
            for half in range(2):
                for fc in range(2):
                    h = fc * 2 + half
                    dve(lambda e, half=half, fc=fc, h=h, at_=at_, tt=tt: e.tensor_tensor(
                        out=at_.ap[:, h, :], in0=apsl[tt][half].ap[:, fc, :], in1=cmaskv, op=ALU.mult),
                        [apsl[tt][half].r(fc), R_CMASK], [at_.r(h)])
        for tt in range(NTT):
            at_ = attnT4[tt]
            for h in range(4):
                fc, half = h // 2, h % 2
                hs = slice(half * 64, (half + 1) * 64)
                op_ = opsl[tt][half]
                mm(op_.ap[:, fc, :], vb.ap[:, tt, h * 128:(h + 1) * 128], at_.ap[:, h, :], True, False,
                   [vb.r(tt), at_.r(h)], [op_.r(fc)])
                for sub in range(2):
                    sc = tt * 2 + sub
                    mm(op_.ap[:, fc, sub * 64:(sub + 1) * 64], Sbf.ap[hs, sc, fc, :],
                       qdec.ap[hs, fc, tt * 128 + sub * 64:tt * 128 + (sub + 1) * 64], False, sub == 1,
                       [Sbf.r(sc), qdec.r(fc, lo=tt * 128, hi=tt * 128 + 128)], [op_.r(fc)])
        for tt in range(NTT):
            for h in range(4):
                fc, half = h // 2, h % 2
                act(oT.ap[:, h, TCS[tt]], opsl[tt][half].ap[:, fc, :], AF.Copy, [opsl[tt][half].r(fc)],
                    [oT.r(h, lo=tt * 128, hi=tt * 128 + 128)])
        sq4 = Buf("sb", sb, sq.off, BF16, (4, T))
        for h in range(4):
            act(sq4.ap[:, h, :], oT.ap[:, h, :], AF.Square, [oT.r(h)], [sq.r(h)])
        for h in range(4):
            mps = psum(4 + h % 4, 0, F32, (T,))
            mm(mps.ap, onesB.ap, sq4.ap[:, h, :], True, True, [onesB.r(), sq.r(h)], [mps.r()])
            act(rstd4.ap[:, h, :], mps.ap, AF.Ln, [mps.r(), R_EPS], [rstd4.r(h)], bias=epsv)
            act(rstd4.ap[:, h, :], rstd4.ap[:, h, :], AF.Exp, [rstd4.r(h)], [rstd4.r(h)], scale=-0.5)
            dve(lambda e, h=h: e.scalar_tensor_tensor(out=oT.ap[:, h, :], in0=oT.ap[:, h, :],
                                                      scalar=cview[:, C_GLAG:C_GLAG + 1], in1=rstd4.ap[:, h, :],
                                                      op0=ALU.mult, op1=ALU.mult),
                [oT.r(h), R_GAIN, rstd4.r(h)], [oT.r(h)])
            dve(lambda e, h=h: e.tensor_tensor(out=ybT.ap[:, h, :], in0=oT.ap[:, h, :], in1=rbT.ap[:, h, :], op=ALU.mult),
                [oT.r(h), rbT.r(h)], [ybT.r(h)])

        for m in range(8):
            wt = wnext("m%d" % m)
            wpa = wt.ap[:, 0:512].rearrange("p (c n) -> p c n", c=4)
            wpb = wt.ap[:, 512:1024].rearrange("p (c n) -> p c n", c=4)
            wga = wt.ap[:, 1024:2048].rearrange("p (k n) -> p k n", k=8)
            wgb = wt.ap[:, 2048:3072].rearrange("p (k n) -> p k n", k=8)
            par = m % 2
            pa = psum(0 + par * 4, 0, F32, (T,))
            pbb = psum(1 + par * 4, 0, F32, (T,))
            ga = psum(2 + par * 4, 0, F32, (T,))
            gb = psum(3 + par * 4, 0, F32, (T,))
            for k in range(8):
                mm(ga.ap, wga[:, k, :], hT.ap[:, k, :], k == 0, k == 7, [wt.r(), hT.r(k)], [ga.r()])
            for k in range(8):
                mm(gb.ap, wgb[:, k, :], hT.ap[:, k, :], k == 0, k == 7, [wt.r(), hT.r(k)], [gb.r()])
            for c in range(4):
                mm(pa.ap, wpa[:, c, :], yaT.ap[:, c, :], c == 0, c == 3, [wt.r(), yaT.r(c)], [pa.r()])
            for c in range(4):
                mm(pbb.ap, wpb[:, c, :], ybT.ap[:, c, :], c == 0, c == 3, [wt.r(), ybT.r(c)], [pbb.r()])
            act(sga[par].ap, ga.ap, AF.Sigmoid, [ga.r()], [sga[par].r()])
            act(sgb[par].ap, gb.ap, AF.Sigmoid, [gb.r()], [sgb[par].r()])
            dve(lambda e, par=par, pa=pa: e.tensor_tensor(out=t1[par].ap, in0=sga[par].ap, in1=pa.ap, op=ALU.mult),
                [sga[par].r(), pa.r()], [t1[par].r()])
            dve(lambda e, par=par, pbb=pbb: e.tensor_tensor(out=t2[par].ap, in0=sgb[par].ap, in1=pbb.ap, op=ALU.mult),
                [sgb[par].r(), pbb.r()], [t2[par].r()])
            dve(lambda e, par=par, m=m: e.tensor_tensor(out=mgT.ap[:, m, :], in0=t1[par].ap, in1=t2[par].ap, op=ALU.add),
                [t1[par].r(), t2[par].r()], [mgT.r(m)])
            wrefill()
        act(dummy.ap[:, 0:1], epsv, AF.Ln, [R_EPS], [dummy.r()])
        for half in range(2):
            wt = wnext("o%d" % half)
            wv = wt.ap.rearrange("p (k n) -> p k n", k=8)
            for i in range(4):
                m = half * 4 + i
                fp = psum(i if half == 0 else 4 + i, 0, F32, (T,))
                for k in range(8):
                    mm(fp.ap, wv[:, k, i * 128:(i + 1) * 128], mgT.ap[:, k, :], k == 0, k == 7,
                       [wt.r(), mgT.r(k)], [fp.r()])
                act(sq.ap[:, m, :], fp.ap, AF.Square, [fp.r()], [sq.r(m)])
                dve(lambda e, m=m, fp=fp: e.tensor_scalar(out=fT.ap[:, m, :], in0=fp.ap,
                                                          scalar1=agc.ap[:, AGI[3], m:m + 1], scalar2=None, op0=ALU.mult),
                    [fp.r(), agc.r()], [fT.r(m)])
            wrefill()
        postnorm_finish(Xb, 3, 1.0)

    def xload(ci):
        Xn = X[ci % 2]
        tr.add("sp", lambda e, ci=ci, Xn=Xn: e.dma_start(out=Xn.ap.rearrange("p a b -> p (a b)"), in_=xh[ci]),
               [], [Xn.r()], dma="D_x%d" % (ci % 2))

    xload(0)
    pre_done = False
    pending_post = None
    for ci in range(nch):
        Xb = X[ci % 2]
        if "ffn1" in stages:
            ffn(Xb, "f1", 0, 1, skip_prenorm=pre_done, drip=pending_post)
        elif pending_post:
            while pending_post:
                pending_post.pop(0)()
        pending_post = None
        if ci + 1 < nch:
            xload(ci + 1)
        pre_done = False
        if "mix" in stages:
            mixer(Xb, ci)

        def store(ci=ci, Xb=Xb):
            tr.add("sp", lambda e: e.dma_start(out=yh[ci], in_=Xb.ap.rearrange("p a b -> p (a b)")),
                   [Xb.r()], [], dma="D_y%d" % (ci % 2))
        if "ffn2" in stages:
            hook = None
            last = not (ci + 1 < nch and "ffn1" in stages and CUT > 3)
            if not last:
                hook = (lambda ci=ci: prenorm(X[(ci + 1) % 2], 0, bank=3))
                pre_done = True
            r_ = ffn(Xb, "f2", 4, 5, mid_hook=hook, defer_post=not last)
            if r_ is not None:
                pending_post = r_ + [store]
            else:
                store()
        else:
            store()
    final_waits = [("D_y0", tr.dma_count.get("D_y0", 0)), ("D_y1", tr.dma_count.get("D_y1", 0))]

    tr.finalize()
    sem_names = ["E_" + e for e in Tracker.ENGS] + sorted(tr.dma_count.keys())
    sems = {}
    import contextlib
    with contextlib.ExitStack() as es_:
        for n in sem_names:
            sems[n] = es_.enter_context(nc.semaphore(n))
        block = es_.enter_context(nc.Block())

        @block.sync
        def _(e):
            tr.emit("sp", e, sems)
            for n, v in final_waits:
                if v > 0:
                    e.wait_ge(sems[n], v)

        @block.gpsimd
        def _(e):
            tr.emit("pool", e, sems)

        @block.tensor
        def _(e):
            tr.emit("pe", e, sems)

        @block.scalar
        def _(e):
            tr.emit("act", e, sems)

        @block.vector
        def _(e):
            tr.emit("dve", e, sems)
    n_ops = {e: len(tr.ops[e]) for e in Tracker.ENGS}
    print("ops per engine:", n_ops)
    return nc


def _bucket(dist):
    n = np.clip(dist, 0, 127)
    max_exact = 16
    nf = np.maximum(n, 1).astype(np.float32)
    large = max_exact + (np.log(nf / np.float32(max_exact)) / np.float32(math.log(128 / max_exact))
                         * np.float32(32 - max_exact)).astype(np.int32)
    large = np.minimum(large, 31)
    return np.where(n < max_exact, n, large)


def pack_weights(inp):
    tiles = np.zeros((NPT, 128, TILE), np.float32)
    names = [n for n, _ in PASS_TILES]

    def put(name, arr):
        a = np.ascontiguousarray(arr).reshape(128, -1)
        tiles[names.index(name), :, :a.shape[1]] = a

    for pfx, key in (("f1", "ffn1"), ("f2", "ffn2")):
        Wg = inp[key + "_w_gate"][0]
        Wu = inp[key + "_w_up"][0]
        Wd = inp[key + "_w_down"][0]
        Wg_r = Wg.reshape(8, 128, NJ, 128).transpose(2, 1, 0, 3)
        Wu_r = Wu.reshape(8, 128, NJ, 128).transpose(2, 1, 0, 3)
        GU = np.stack([Wg_r, Wu_r], axis=2)
        for i in range(11):
            t_ = GU[2 * i:2 * i + 2].transpose(1, 0, 2, 3, 4)
            put(pfx + "gu%d" % i, t_)
        Wd_r = Wd.reshape(NJ, 128, 2, 512)
        for half in range(2):
            for ti in range(3):
                js = list(range(ti * 8, min(NJ, ti * 8 + 8)))
                t_ = Wd_r[js, :, half, :].transpose(1, 0, 2)
                put(pfx + "d%d" % (half * 3 + ti), t_)
    w_in = inp["w_in"][0]
    qperm = np.array([(half * 4 + c) * 64 + d for c in range(4) for half in range(2) for d in range(64)])

    def kmaj(cols):
        return cols.reshape(8, 128, -1).transpose(1, 0, 2)

    put("a0", kmaj(w_in[:, 0:512][:, qperm]))
    put("a1", kmaj(np.concatenate([w_in[:, 512:640], w_in[:, 640:768], w_in[:, 768:1024]], axis=1)))
    put("a2", kmaj(w_in[:, 1280:1792]))
    put("a3", kmaj(w_in[:, 1792:2304]))
    a4 = np.zeros((1024, 512), np.float32)
    a4[:, 0:256] = w_in[:, 1024:1280]
    a4[:, 256:272] = w_in[:, 2304:2320]
    put("a4", kmaj(a4))
    wpa = inp["w_proj_a"][0][qperm, :]
    wpb = inp["w_proj_b"][0]
    wga = w_in[:, 2320:3344]
    wgb = w_in[:, 3344:4368]
    for m in range(8):
        cs = slice(m * 128, (m + 1) * 128)
        parts = [wpa[:, cs].reshape(4, 128, 128).transpose(1, 0, 2).reshape(128, -1),
                 wpb[:, cs].reshape(4, 128, 128).transpose(1, 0, 2).reshape(128, -1),
                 wga[:, cs].reshape(8, 128, 128).transpose(1, 0, 2).reshape(128, -1),
                 wgb[:, cs].reshape(8, 128, 128).transpose(1, 0, 2).reshape(128, -1)]
        put("m%d" % m, np.concatenate(parts, axis=1))
    wo = inp["w_out"][0]
    for half in range(2):
        put("o%d" % half, kmaj(wo[:, half * 512:(half + 1) * 512]))
    return tiles


def pack_consts(inp):
    c = np.zeros((128, C_TOT), np.float32)
    gl = [inp["ffn1_pre_g"][0], inp["ffn1_post_g"][0], inp["mix_pre_g"][0], inp["mix_post_g"][0],
          inp["ffn2_pre_g"][0], inp["ffn2_post_g"][0]]
    for n, g in enumerate(gl):
        c[:, C_GAIN + n * 8:C_GAIN + (n + 1) * 8] = g.reshape(8, 128).T
    c[:, C_GLAG] = inp["gla_norm_g"][0]
    c[:, C_SINK:C_SINK + 8] = np.broadcast_to(inp["attn_sinks"][0][None, :], (128, 8))
    t_idx = np.arange(128)[:, None]
    j_idx = np.arange(256)[None, :]
    dist = t_idx + 128 - j_idx
    bk = _bucket(dist)
    bias = inp["rel_bias"][bk]
    c[:, C_BIAS:C_BIAS + 2048] = bias.transpose(0, 2, 1).reshape(128, 2048)
    valid = (dist >= 0) & (dist < 128)
    c[:, C_MASK:C_MASK + 256] = np.where(valid, 0.0, -1e30).astype(np.float32)
    c[:, C_IDENT:C_IDENT + 128] = np.eye(128, dtype=np.float32)
    s_i = np.arange(128)[:, None]
    t_i = np.arange(128)[None, :]
    same = (s_i // 64) == (t_i // 64)
    c[:, C_TRI:C_TRI + 128] = np.where(same & (s_i <= t_i), -1.0 / 16.0, 0.0)
    c[:, C_CMASK:C_CMASK + 128] = np.where(same & (s_i <= t_i), 1.0, 0.0)
    c[0:16, C_WAUG:C_WAUG + 256] = inp["w_alpha"][0]
    c[16, C_WAUG:C_WAUG + 256] = inp["b_alpha"][0]
    c[:, C_EPS] = EPS
    return c


_CACHE = {}


def kernel(**inputs):
    inp = {k: np.asarray(v) for k, v in inputs.items()}
    stages = tuple(os.environ.get("MK_STAGES", "ffn1,mix,ffn2").split(","))
    x = inp["x"]
    xr = x.reshape(NCORES, 2, 4, T, 8, 128)
    xh = np.ascontiguousarray(xr.transpose(0, 1, 2, 5, 4, 3)).reshape(NCORES, NCH, 128, 8 * T)
    wt = pack_weights(inp)
    cs = pack_consts(inp)
    nc = build_program(stages)
    in_maps = [{"xh": xh[i], "wst": wt, "cst": cs} for i in range(NCORES)]
    res = run_bass_kernel_spmd(nc, in_maps, core_ids=list(range(NCORES)))
    yh = np.stack([np.asarray(r["yh"]) for r in res.results], axis=0)
    y = yh.reshape(NCORES, 2, 4, 128, 8, T).transpose(0, 1, 2, 5, 4, 3).reshape(16, 2048, 1024)
    return np.ascontiguousarray(y.astype(np.float32))
```

```python
import os
import math
import numpy as np
import concourse.bass as bass
import concourse.mybir as mybir
from concourse.bass_utils import run_bass_kernel_spmd

F32 = mybir.dt.float32
BF16 = mybir.dt.bfloat16
AF = mybir.ActivationFunctionType
ALU = mybir.AluOpType
AX = mybir.AxisListType

D = 1024
DFF = 2816
NJ = 22
T = 512
NTT = 4
NCH = 8
NCORES = 8
EPS = 1e-6
RING = 5
TILE = 4096

GU_T = [("gu%d" % i, 4096) for i in range(11)]
D_T = [("d%d" % i, 4096) for i in range(6)]
MIX_T = [("a0", 4096), ("a1", 4096), ("a2", 4096), ("a3", 4096), ("a4", 8 * 512)] + \
        [("m%d" % m, 3072) for m in range(8)] + [("o0", 4096), ("o1", 4096)]
PASS_TILES = [("f1" + n, u) for n, u in GU_T + D_T] + MIX_T + [("f2" + n, u) for n, u in GU_T + D_T]
NPT = len(PASS_TILES)

C_GAIN = 0
C_GLAG = 48
C_SINK = 49
C_BIAS = 57
C_MASK = C_BIAS + 2048
C_IDENT = C_MASK + 256
C_TRI = C_IDENT + 128
C_CMASK = C_TRI + 128
C_WAUG = C_CMASK + 128
C_EPS = C_WAUG + 256
C_TOT = C_EPS + 1


class Op:
    __slots__ = ("eng", "fn", "deps", "signal", "sigval", "dma", "idx")

    def __init__(self, eng, fn, dma=None):
        self.eng = eng
        self.fn = fn
        self.deps = []
        self.signal = False
        self.sigval = 0
        self.dma = dma


class Tracker:
    ENGS = ("pe", "act", "dve", "pool", "sp")

    def __init__(self):
        self.ops = {e: [] for e in self.ENGS}
        self.segs = {}
        self.dma_count = {}

    def _dep(self, cons, prod, kind):
        if prod is None or prod is cons:
            return
        if prod.dma is None:
            if prod.eng == cons.eng:
                if prod.eng == "pe":
                    return
            prod.signal = True
        cons.deps.append(prod)

    def _touch(self, op, space, lo, hi, write):
        segs = self.segs.setdefault(space, [])
        new = []
        cur = lo
        out = []
        for s in segs:
            if s[1] <= lo or s[0] >= hi:
                out.append(s)
                continue
            if s[0] < lo:
                out.append([s[0], lo, s[2], dict(s[3]), list(s[4])])
                s = [lo, s[1], s[2], s[3], s[4]]
            if s[1] > hi:
                out.append([hi, s[1], s[2], dict(s[3]), list(s[4])])
                s = [s[0], hi, s[2], s[3], s[4]]
            new.append(s)
        new.sort(key=lambda s: s[0])
        filled = []
        for s in new:
            if s[0] > cur:
                filled.append([cur, s[0], None, {}, []])
            filled.append(s)
            cur = s[1]
        if cur < hi:
            filled.append([cur, hi, None, {}, []])
        for s in filled:
            if write:
                self._dep(op, s[2], "waw")
                for r in s[3].values():
                    self._dep(op, r, "war")
                for r in s[4]:
                    self._dep(op, r, "war")
                s[2] = op
                s[3] = {}
                s[4] = []
            else:
                self._dep(op, s[2], "raw")
                if op.dma is not None:
                    s[4].append(op)
                else:
                    s[3][op.eng] = op
        if write:
            filled = [[lo, hi, op, {}, []]]
        out.extend(filled)
        self.segs[space] = out

    def add(self, eng, fn, reads=(), writes=(), dma=None):
        d = None
        if dma is not None:
            self.dma_count[dma] = self.dma_count.get(dma, 0) + 16
            d = [dma, self.dma_count[dma]]
        op = Op(eng, fn, d)
        for (sp, lo, hi) in writes:
            if sp == "ps":
                self._touch(op, sp, lo // 2048 * 2048, (hi + 2047) // 2048 * 2048, True)
            else:
                self._touch(op, sp, lo, hi, True)
        for (sp, lo, hi) in reads:
            if sp == "ps":
                self._touch(op, sp, lo // 2048 * 2048, (hi + 2047) // 2048 * 2048, True)
            else:
                self._touch(op, sp, lo, hi, False)
        self.ops[eng].append(op)
        return op

    def finalize(self):
        for e in self.ENGS:
            k = 0
            for op in self.ops[e]:
                if op.signal and op.dma is None:
                    k += 1
                    op.sigval = k

    def emit(self, eng_name, eng, sems):
        waited = {}
        for op in self.ops[eng_name]:
            need = {}
            for p in op.deps:
                if p.dma is not None:
                    key, val = p.dma[0], p.dma[1]
                else:
                    key, val = "E_" + p.eng, p.sigval
                if val > need.get(key, 0):
                    need[key] = val
            for key, val in need.items():
                if waited.get(key, 0) < val:
                    eng.wait_ge(sems[key], val)
                    waited[key] = val
            ins = op.fn(eng)
            if op.dma is not None:
                ins.then_inc(sems[op.dma[0]], 16)
            elif op.signal:
                ins.then_inc(sems["E_" + eng_name], 1)


class Buf:
    def __init__(self, space, base, off_bytes, dtype, shape, npart=128):
        self.space = space
        self.off = off_bytes
        self.es = 2 if dtype == BF16 else 4
        self.shape = tuple(shape)
        n = int(np.prod(shape))
        self.n = n
        w0 = off_bytes // 4
        w1 = (off_bytes + n * self.es + 3) // 4
        v = base[:, w0:w1]
        if dtype != F32:
            v = v.bitcast(dtype)
        if len(shape) == 2:
            v = v.rearrange("p (a b) -> p a b", a=shape[0])
        elif len(shape) == 3:
            v = v.rearrange("p (a b c) -> p a b c", a=shape[0], b=shape[1])
        self.ap = v

    def r(self, *idx, lo=None, hi=None):
        stride = self.n
        off = 0
        for i, ix in enumerate(idx):
            stride //= self.shape[i]
            off += ix * stride
        a = 0 if lo is None else lo
        b = stride if hi is None else hi
        return (self.space, self.off + (off + a) * self.es, self.off + (off + b) * self.es)


def build_program(stages=("ffn1", "mix", "ffn2"), nch=NCH):
    nc = bass.Bass("TRN2", target_bir_lowering=False)
    xh = nc.dram_tensor("xh", [NCH, 128, 8 * T], F32, kind="ExternalInput").ap()
    wst = nc.dram_tensor("wst", [NPT, 128, TILE], F32, kind="ExternalInput").ap()
    cst = nc.dram_tensor("cst", [128, C_TOT], F32, kind="ExternalInput").ap()
    yh = nc.dram_tensor("yh", [NCH, 128, 8 * T], F32, kind="ExternalOutput").ap()

    SBW = 53000
    sb = nc.alloc_sbuf_tensor("sb", [128, SBW], F32)
    ps = nc.alloc_psum_tensor("ps", [128, 4096], F32)
    tr = Tracker()
    alloc = {"off": 0}

    def sbuf(dtype, shape):
        es = 2 if dtype == BF16 else 4
        n = int(np.prod(shape)) * es
        n = (n + 63) // 64 * 64
        b = Buf("sb", sb, alloc["off"], dtype, shape)
        alloc["off"] += n
        assert alloc["off"] <= SBW * 4, alloc["off"]
        return b

    def psum(bank, off_words, dtype, shape):
        return Buf("ps", ps, (bank * 512 + off_words) * 4, dtype, shape)

    def region(nbytes):
        o = alloc["off"]
        alloc["off"] += (nbytes + 63) // 64 * 64
        assert alloc["off"] <= SBW * 4, alloc["off"]
        return {"base": o, "off": o, "end": o + nbytes}

    def rsub(reg, dtype, shape):
        es = 2 if dtype == BF16 else 4
        n = (int(np.prod(shape)) * es + 63) // 64 * 64
        b = Buf("sb", sb, reg["off"], dtype, shape)
        reg["off"] += n
        assert reg["off"] <= reg["end"], (reg, n)
        return b

    def rreset(reg):
        reg["off"] = reg["base"]

    X = [sbuf(F32, (8, T)) for _ in range(2)]
    hT = sbuf(BF16, (8, T))
    sq = sbuf(BF16, (8, T))
    rstd = sbuf(F32, (T,))
    ring = [sbuf(BF16, (TILE,)) for _ in range(RING)]
    CST = sbuf(F32, (C_TOT,))
    ident = sbuf(BF16, (128,))
    onesA = sbuf(BF16, (128,))
    onesB = sbuf(BF16, (128,))
    nsink = sbuf(F32, (8,))
    gs = [sbuf(BF16, (T,)) for _ in range(2)]
    agc = sbuf(F32, (3, 8))
    dummy = sbuf(F32, (2,))
    kaT = sbuf(BF16, (128 + T,))
    va = sbuf(BF16, (5, 128))
    alrT = sbuf(F32, (T,))
    yaT = sbuf(BF16, (4, T))
    ybT = sbuf(BF16, (4, T))
    pT = [sbuf(BF16, (8, 128)) for _ in range(2)]
    st_mx = [sbuf(F32, (4,)) for _ in range(2)]
    st_negm = [sbuf(F32, (4,)) for _ in range(2)]
    st_rsum = [sbuf(F32, (4,)) for _ in range(2)]
    st_t = [sbuf(F32, (4,)) for _ in range(2)]
    st_es = [sbuf(F32, (4,)) for _ in range(2)]
    st_rden = [sbuf(F32, (4,)) for _ in range(2)]
    e1 = sbuf(F32, (256,))
    spb = sbuf(F32, (256,))
    eb = sbuf(F32, (2, 128))
    enb = sbuf(F32, (2, 128))
    eks = sbuf(F32, (2, 128))
    blast = sbuf(F32, (2, 2))
    decay = sbuf(F32, (2, 2))
    attnT = [sbuf(BF16, (4, 128)) for _ in range(2)]
    S = sbuf(F32, (2, 128))
    S_alt = sbuf(F32, (2, 128))
    Sbf = sbuf(BF16, (8, 2, 128))
    regA = region(NJ * T * 2)
    actT = rsub(regA, BF16, (NJ, T))
    rreset(regA)
    qaT = rsub(regA, BF16, (4, T))
    qbT = rsub(regA, F32, (2, T))
    kbT = rsub(regA, F32, (2, T))
    vb = rsub(regA, BF16, (4, 512))
    rbT = rsub(regA, BF16, (4, T))
    regF = region(8 * T * 4)
    fT = rsub(regF, F32, (8, T))
    rreset(regF)
    oT = rsub(regF, F32, (4, T))
    rstd4 = rsub(regF, F32, (4, T))
    regS = region(16384)
    s_sb = [rsub(regS, F32, (4, 256)) for _ in range(2)]
    p_sb = [rsub(regS, BF16, (4, 256)) for _ in range(2)]
    pn_sb = [rsub(regS, BF16, (4, 256)) for _ in range(2)]
    rreset(regS)
    sga = [rsub(regS, F32, (T,)) for _ in range(2)]
    sgb = [rsub(regS, F32, (T,)) for _ in range(2)]
    t1 = [rsub(regS, F32, (T,)) for _ in range(2)]
    t2 = [rsub(regS, F32, (T,)) for _ in range(2)]
    rreset(regS)
    spb4 = [rsub(regS, F32, (256,)) for _ in range(4)]
    eb4 = [rsub(regS, F32, (2, 128)) for _ in range(4)]
    enb4 = [rsub(regS, F32, (2, 128)) for _ in range(4)]
    eks4 = [rsub(regS, F32, (2, 128)) for _ in range(4)]
    attnT4 = attnT + [sbuf(BF16, (4, 128)) for _ in range(2)]
    blast4 = [sbuf(F32, (2, 2)) for _ in range(4)]
    decay4 = [sbuf(F32, (2, 2)) for _ in range(4)]
    regM = region(8 * T * 2)
    mgT = rsub(regM, BF16, (8, T))
    rreset(regM)
    qdec = rsub(regM, BF16, (2, T))
    kin = rsub(regM, BF16, (2, T))
    kst = rsub(regM, BF16, (2, T))
    kst_tok = rsub(regM, BF16, (4, 256))
    print("SBUF bytes/partition used:", alloc["off"])

    def mm(out_ap, lhsT, rhs, start, stop, reads, writes):
        tr.add("pe", lambda e: e.matmul(out_ap, lhsT, rhs, start=start, stop=stop), reads, writes)

    def tp(out_ap, in_ap, reads, writes):
        tr.add("pe", lambda e: e.transpose(out_ap, in_ap, ident.ap[:, :]), reads + [ident.r()], writes)

    def act(out_ap, in_ap, func, reads, writes, bias=None, scale=None, accum=None):
        kw = {}
        if bias is not None:
            kw["bias"] = bias
        if scale is not None:
            kw["scale"] = scale
        if accum is not None:
            kw["accum_out"] = accum
        tr.add("act", lambda e: e.activation(out_ap, in_ap, func, **kw), reads, writes)

    def dve(fn, reads, writes):
        tr.add("dve", fn, reads, writes)

    tr.add("sp", lambda e: e.dma_start(out=CST.ap, in_=cst), [], [CST.r()], dma="D_cst")
    cview = CST.ap
    dve(lambda e: e.tensor_copy(out=ident.ap, in_=cview[:, C_IDENT:C_IDENT + 128]),
        [CST.r(lo=C_IDENT, hi=C_IDENT + 128)], [ident.r()])
    dve(lambda e: e.memset(onesA.ap, 1.0 / 1024.0), [], [onesA.r()])
    dve(lambda e: e.memset(onesB.ap, 1.0 / 128.0), [], [onesB.r()])
    dve(lambda e: e.tensor_scalar(out=nsink.ap, in0=cview[:, C_SINK:C_SINK + 8], scalar1=-1.0, scalar2=None,
                                  op0=ALU.mult),
        [CST.r(lo=C_SINK, hi=C_SINK + 8)], [nsink.r()])
    biasv = cview[:, C_BIAS:C_BIAS + 2048].rearrange("p (h s) -> p h s", h=8)
    maskv = cview[:, C_MASK:C_MASK + 256]
    dve(lambda e: e.tensor_tensor(out=biasv, in0=biasv, in1=maskv.unsqueeze(1).to_broadcast([128, 8, 256]),
                                  op=ALU.add),
        [CST.r(lo=C_BIAS, hi=C_MASK + 256)], [CST.r(lo=C_BIAS, hi=C_BIAS + 2048)])
    dve(lambda e: e.memset(alrT.ap[0:32, :], 1.0), [], [alrT.r()])
    for i_, (gi_, al_) in enumerate(((1, 0.5), (3, 1.0), (5, 0.5))):
        dve(lambda e, i_=i_, gi_=gi_, al_=al_: e.tensor_scalar(
            out=agc.ap[:, i_, :], in0=cview[:, C_GAIN + gi_ * 8:C_GAIN + gi_ * 8 + 8], scalar1=float(al_), scalar2=None,
            op0=ALU.mult), [CST.r(lo=0, hi=57)], [agc.r(i_)])
    AGI = {1: 0, 3: 1, 5: 2}
    triv = cview[:, C_TRI:C_TRI + 128]
    cmaskv = cview[:, C_CMASK:C_CMASK + 128]
    waugv = cview[0:32, C_WAUG:C_WAUG + 256]
    R_TRI = CST.r(lo=C_TRI, hi=C_TRI + 128)
    R_CMASK = CST.r(lo=C_CMASK, hi=C_CMASK + 128)
    R_WAUG = CST.r(lo=C_WAUG, hi=C_WAUG + 256)
    R_EPS = CST.r(lo=C_EPS, hi=C_EPS + 1)
    epsv = cview[:, C_EPS:C_EPS + 1]

    def gain(n, c):
        return cview[:, C_GAIN + n * 8 + c:C_GAIN + n * 8 + c + 1]

    R_GAIN = CST.r(lo=0, hi=57)

    wstate = {"g": 0}

    def wload(pidx):
        g = wstate["g"]
        wstate["g"] += 1
        slot = g % RING
        used = PASS_TILES[pidx][1]
        b = ring[slot]
        tr.add("pool", lambda e: e.dma_start(out=b.ap[:, 0:used], in_=wst[pidx][:, 0:used]),
               [], [b.r()], dma="D_w%d" % slot)
        return b

    pending = []
    CUT_ = int(os.environ.get("MK_CUT", "99"))
    order = {"pos": 0, "seq": []}
    for ci in range(nch):
        for pidx in range(NPT):
            nm = PASS_TILES[pidx][0]
            if nm.startswith("f1") and "ffn1" not in stages:
                continue
            if nm.startswith("f2") and "ffn2" not in stages:
                continue
            if not (nm.startswith("f1") or nm.startswith("f2")) and "mix" not in stages:
                continue
            if nm[:2] in ("f1", "f2"):
                if CUT_ <= 1:
                    continue
                if CUT_ <= 2 and nm[2] == "d":
                    continue
            order["seq"].append(pidx)

    def wnext(expect_name):
        while len(pending) < RING and order["pos"] < len(order["seq"]):
            pidx = order["seq"][order["pos"]]
            order["pos"] += 1
            pending.append((pidx, wload(pidx)))
        pidx, b = pending.pop(0)
        assert PASS_TILES[pidx][0] == expect_name, (PASS_TILES[pidx][0], expect_name)
        return b

    def wrefill():
        while len(pending) < RING - 1 and order["pos"] < len(order["seq"]):
            pidx = order["seq"][order["pos"]]
            order["pos"] += 1
            pending.append((pidx, wload(pidx)))

    def rms_stats(bank):
        mps = psum(bank, 0, F32, (T,))
        for c in range(8):
            mm(mps.ap, onesA.ap, sq.ap[:, c, :], c == 0, c == 7,
               [onesA.r(), sq.r(c)], [mps.r()])
        act(rstd.ap, mps.ap, AF.Ln, [mps.r(), R_EPS], [rstd.r()], bias=epsv)
        act(rstd.ap, rstd.ap, AF.Exp, [rstd.r()], [rstd.r()], scale=-0.5)

    def prenorm(Xb, gidx, bank=0):
        for c in range(8):
            act(sq.ap[:, c, :], Xb.ap[:, c, :], AF.Square, [Xb.r(c)], [sq.r(c)])
        rms_stats(bank)
        for c in range(8):
            dve(lambda e, c=c: e.scalar_tensor_tensor(out=hT.ap[:, c, :], in0=Xb.ap[:, c, :], scalar=gain(gidx, c),
                                                      in1=rstd.ap, op0=ALU.mult, op1=ALU.mult),
                [Xb.r(c), R_GAIN, rstd.r()], [hT.r(c)])

    def postnorm(Xb, gidx, alpha, fbanks):
        for m in range(8):
            fp = fbanks[m]
            act(sq.ap[:, m, :], fp.ap, AF.Square, [fp.r()], [sq.r(m)])
            dve(lambda e, m=m, fp=fp: e.tensor_copy(out=fT.ap[:, m, :], in_=fp.ap), [fp.r()], [fT.r(m)])

    def postnorm_ops(Xb, bank):
        def stats():
            mps = psum(bank, 0, F32, (T,))
            for c in range(8):
                mm(mps.ap, onesA.ap, sq.ap[:, c, :], c == 0, c == 7, [onesA.r(), sq.r(c)], [mps.r()])
            act(rstd.ap, mps.ap, AF.Ln, [mps.r(), R_EPS], [rstd.r()], bias=epsv)
        ops = [stats,
               lambda: act(rstd.ap, rstd.ap, AF.Exp, [rstd.r()], [rstd.r()], scale=-0.5)]
        def mul_(m):
            return lambda: dve(lambda e: e.tensor_tensor(out=fT.ap[:, m, :], in0=fT.ap[:, m, :], in1=rstd.ap,
                                                         op=ALU.mult), [fT.r(m), rstd.r()], [fT.r(m)])

        def add_(m):
            return lambda: dve(lambda e: e.tensor_tensor(out=Xb.ap[:, m, :], in0=Xb.ap[:, m, :], in1=fT.ap[:, m, :],
                                                         op=ALU.add), [fT.r(m), Xb.r(m)], [Xb.r(m)])
        ops.append(mul_(0))
        for m in range(1, 8):
            ops.append(mul_(m))
            ops.append(add_(m - 1))
        ops.append(add_(7))
        return ops

    def postnorm_finish(Xb, gidx, alpha, bank=0):
        for f_ in postnorm_ops(Xb, bank):
            f_()

    CUT = int(os.environ.get("MK_CUT", "99"))

    def ffn(Xb, pfx, g_pre, g_post, skip_prenorm=False, mid_hook=None, drip=None, defer_post=False):
        if CUT <= 0:
            return
        if not skip_prenorm:
            prenorm(Xb, g_pre)
        if CUT <= 1:
            return
        gps = [psum(0, 0, F32, (T,)), psum(1, 0, F32, (T,))]
        ups = [psum(2, 0, F32, (T,)), psum(3, 0, F32, (T,))]
        wt = None
        for j in range(NJ):
            if j % 2 == 0:
                wt = wnext(pfx + "gu%d" % (j // 2))
                wv = wt.ap.rearrange("p (jj gu k c) -> p jj gu k c", jj=2, gu=2, k=8)
            jj = j % 2
            gp, up = gps[j % 2], ups[j % 2]
            if j == 0:
                for k in range(8):
                    for j2 in range(2):
                        mm(gps[j2].ap, wv[:, j2, 0, k, :], hT.ap[:, k, :], k == 0, k == 7, [wt.r(), hT.r(k)], [gps[j2].r()])
                        mm(ups[j2].ap, wv[:, j2, 1, k, :], hT.ap[:, k, :], k == 0, k == 7, [wt.r(), hT.r(k)], [ups[j2].r()])
            elif j >= 2:
                for k in range(8):
                    mm(gp.ap, wv[:, jj, 0, k, :], hT.ap[:, k, :], k == 0, k == 7, [wt.r(), hT.r(k)], [gp.r()])
                for k in range(8):
                    mm(up.ap, wv[:, jj, 1, k, :], hT.ap[:, k, :], k == 0, k == 7, [wt.r(), hT.r(k)], [up.r()])
            g_ = gs[j % 2]
            act(g_.ap, gp.ap, AF.Silu, [gp.r()], [g_.r()])
            dve(lambda e, j=j, g_=g_, up=up: e.tensor_tensor(out=actT.ap[:, j, :], in0=g_.ap, in1=up.ap, op=ALU.mult),
                [g_.r(), up.r()], [actT.r(j)])
            if drip:
                for _ in range(3 if j > 0 else 1):
                    if drip:
                        drip.pop(0)()
            wrefill()
        while drip:
            drip.pop(0)()
        if CUT <= 2:
            return
        if mid_hook is not None:
            mid_hook()
        else:
            act(dummy.ap[:, 0:1], epsv, AF.Ln, [R_EPS], [dummy.r()])
        for half in range(2):
            fps = [psum((4 if half == 0 else 0) + i, 0, F32, (T,)) for i in range(4)]
            for ti in range(3):
                wt = wnext(pfx + "d%d" % (half * 3 + ti))
                wv = wt.ap.rearrange("p (jj n) -> p jj n", jj=8)
                for jj in range(8):
                    j = ti * 8 + jj
                    if j >= NJ:
                        break
                    for i in range(4):
                        mm(fps[i].ap, wv[:, jj, i * 128:(i + 1) * 128], actT.ap[:, j, :], j == 0, j == NJ - 1,
                           [wt.r(), actT.r(j)], [fps[i].r()])
                wrefill()
            for i in range(4):
                m = half * 4 + i
                fp = fps[i]
                act(sq.ap[:, m, :], fp.ap, AF.Square, [fp.r()], [sq.r(m)])
                dve(lambda e, m=m, fp=fp: e.tensor_scalar(out=fT.ap[:, m, :], in0=fp.ap,
                                                          scalar1=agc.ap[:, AGI[g_post], m:m + 1], scalar2=None, op0=ALU.mult),
                    [fp.r(), agc.r()], [fT.r(m)])
        if CUT <= 3:
            return
        if defer_post:
            ops_ = postnorm_ops(Xb, 4)
            ops_.pop(0)()
            return ops_
        postnorm_finish(Xb, g_post, 0.5, bank=4)
        return None

    def proj_fm(wt, wv, col0, ncols, bank, evac):
        pb = psum(bank, 0, F32, (T,))
        for k in range(8):
            mm(pb.ap[0:ncols, :], wv[:, k, col0:col0 + ncols], hT.ap[:, k, :], k == 0, k == 7,
               [wt.r(), hT.r(k)], [pb.r()])
        evac(pb)

    def mixer(Xb, ci):
        first_of_seq = (ci % 4 == 0)
        prenorm(Xb, 2)
        bank = {"i": 0}

        def nb():
            b = bank["i"]
            bank["i"] = (b + 1) % 4
            return b

        wt = wnext("a0")
        wv = wt.ap.rearrange("p (k c) -> p k c", k=8)
        qps = [psum(c, 0, F32, (T,)) for c in range(4)]
        for k in range(8):
            for c in range(4):
                mm(qps[c].ap, wv[:, k, c * 128:(c + 1) * 128], hT.ap[:, k, :], k == 0, k == 7,
                   [wt.r(), hT.r(k)], [qps[c].r()])
        for c in range(4):
            act(qaT.ap[:, c, :], qps[c].ap, AF.Copy, [qps[c].r()], [qaT.r(c)])
        wrefill()
        wt = wnext("a1")
        wv = wt.ap.rearrange("p (k c) -> p k c", k=8)
        proj_fm(wt, wv, 0, 128, nb(),
                lambda pb: act(kaT.ap[:, 128:128 + T], pb.ap, AF.Copy, [pb.r()], [kaT.r(lo=128, hi=128 + T)]))
        pbv = psum(nb(), 0, F32, (4, 128))
        for tt in range(NTT):
            for k in range(8):
                mm(pbv.ap[:, tt, :], hT.ap[:, k, tt * 128:(tt + 1) * 128], wv[:, k, 128:256], k == 0, k == 7,
                   [wt.r(), hT.r(k)], [pbv.r(tt)])
        act(va.ap[:, 1:5, :], pbv.ap, AF.Copy, [pbv.r()], [va.r(lo=128, hi=640)])
        for c in range(2):
            proj_fm(wt, wv, 256 + c * 128, 128, nb(),
                    lambda pb, c=c: dve(lambda e: e.tensor_copy(out=qbT.ap[:, c, :], in_=pb.ap), [pb.r()], [qbT.r(c)]))
        wrefill()
        wt = wnext("a2")
        wv = wt.ap.rearrange("p (k c) -> p k c", k=8)
        for tt in range(NTT):
            pb = psum(nb(), 0, F32, (T,))
            for k in range(8):
                mm(pb.ap, hT.ap[:, k, tt * 128:(tt + 1) * 128], wv[:, k, :], k == 0, k == 7,
                   [wt.r(), hT.r(k)], [pb.r()])
            act(vb.ap[:, tt, :], pb.ap, AF.Copy, [pb.r()], [vb.r(tt)])
        wrefill()
        wt = wnext("a3")
        wv = wt.ap.rearrange("p (k c) -> p k c", k=8)
        for c in range(4):
            proj_fm(wt, wv, c * 128, 128, nb(),
                    lambda pb, c=c: act(rbT.ap[:, c, :], pb.ap, AF.Silu, [pb.r()], [rbT.r(c)]))
        wrefill()
        wt = wnext("a4")
        wv = wt.ap.rearrange("p (k c) -> p k c", k=8)
        for c in range(2):
            proj_fm(wt, wv, c * 128, 128, nb(),
                    lambda pb, c=c: dve(lambda e: e.tensor_copy(out=kbT.ap[:, c, :], in_=pb.ap), [pb.r()], [kbT.r(c)]))
        proj_fm(wt, wv, 256, 16, nb(),
                lambda pb: dve(lambda e: e.tensor_copy(out=alrT.ap[0:16, :], in_=pb.ap[0:16, :]), [pb.r()], [alrT.r()]))
        wrefill()

        H2 = (0, 1)
        spsl = [psum(0, 0, F32, (4, 256)), psum(2, 0, F32, (4, 256))]
        tpsl = [psum(4, 0, BF16, (8, 128)), psum(5, 0, BF16, (8, 128))]
        hsl = [slice(0, 64), slice(64, 128)]
        yps = psum(6, 0, F32, (4, 128))

        def blk_params(tt):
            first_blk = first_of_seq and tt == 0
            ncol = 128 if first_blk else 256
            boff = 128 if first_blk else 0
            kc0 = tt * 128 + boff
            blks = [1] if first_blk else [0, 1]
            return first_blk, ncol, boff, kc0, blks

        def swa_scores(tt):
            first_blk, ncol, boff, kc0, blks = blk_params(tt)
            for half in H2:
                for c in range(4):
                    mm(spsl[half].ap[:, c, 0:ncol], qaT.ap[hsl[half], c, tt * 128:(tt + 1) * 128],
                       kaT.ap[hsl[half], kc0:kc0 + ncol],
                       True, True, [qaT.r(c), kaT.r(lo=kc0, hi=kc0 + ncol)], [spsl[half].r(c)])

        def swa_chain_a(tt):
            first_blk, ncol, boff, kc0, blks = blk_params(tt)
            for half in H2:
                dve(lambda e, sps=spsl[half], s_=s_sb[half], half=half, ncol=ncol, boff=boff: e.scalar_tensor_tensor(
                    out=s_.ap[:, :, 0:ncol], in0=sps.ap[:, :, 0:ncol], scalar=0.125,
                    in1=biasv[:, half * 4:half * 4 + 4, boff:boff + ncol], op0=ALU.mult, op1=ALU.add),
                    [spsl[half].r(), CST.r(lo=C_BIAS, hi=C_BIAS + 2048)], [s_sb[half].r()])
                dve(lambda e, s_=s_sb[half], mx=st_mx[half], ncol=ncol: e.tensor_reduce(
                    out=mx.ap, in_=s_.ap[:, :, 0:ncol], axis=AX.X, op=ALU.max),
                    [s_sb[half].r()], [st_mx[half].r()])
                dve(lambda e, mx=st_mx[half], negm=st_negm[half], half=half: e.scalar_tensor_tensor(
                    out=negm.ap, in0=mx.ap, scalar=-1.0, in1=nsink.ap[:, half * 4:half * 4 + 4],
                    op0=ALU.mult, op1=ALU.min),
                    [st_mx[half].r(), nsink.r()], [st_negm[half].r()])
                dve(lambda e, tt_=st_t[half], negm=st_negm[half], half=half: e.tensor_tensor(
                    out=tt_.ap, in0=negm.ap, in1=cview[:, C_SINK + half * 4:C_SINK + half * 4 + 4], op=ALU.add),
                    [st_negm[half].r(), CST.r(lo=C_SINK, hi=C_SINK + 8)], [st_t[half].r()])
                p_, s_, negm, rsum = p_sb[half], s_sb[half], st_negm[half], st_rsum[half]
                for c in range(4):
                    act(p_.ap[:, c, 0:ncol], s_.ap[:, c, 0:ncol], AF.Exp, [s_.r(c), negm.r()], [p_.r(c), rsum.r()],
                        bias=negm.ap[:, c:c + 1], accum=rsum.ap[:, c:c + 1])
                act(st_es[half].ap, st_t[half].ap, AF.Exp, [st_t[half].r()], [st_es[half].r()])

        def swa_chain_b(tt):
            first_blk, ncol, boff, kc0, blks = blk_params(tt)
            for half in H2:
                dve(lambda e, es=st_es[half], rsum=st_rsum[half]: e.tensor_tensor(out=es.ap, in0=es.ap, in1=rsum.ap, op=ALU.add),
                    [st_es[half].r(), st_rsum[half].r()], [st_es[half].r()])
                dve(lambda e, es=st_es[half], rden=st_rden[half]: e.reciprocal(out=rden.ap, in_=es.ap),
                    [st_es[half].r()], [st_rden[half].r()])
                p_, pn_, rden = p_sb[half], pn_sb[half], st_rden[half]
                for c in range(4):
                    dve(lambda e, p_=p_, pn_=pn_, rden=rden, c=c, ncol=ncol: e.tensor_scalar(
                        out=pn_.ap[:, c, 0:ncol], in0=p_.ap[:, c, 0:ncol], scalar1=rden.ap[:, c:c + 1], scalar2=None,
                        op0=ALU.mult),
                        [p_.r(c), rden.r()], [pn_.r(c)])
            for half in H2:
                tps, pn_ = tpsl[half], pn_sb[half]
                for c in range(4):
                    for bi, blk in enumerate(blks):
                        tp(tps.ap[:, c * 2 + blk, :], pn_.ap[:, c, bi * 128:(bi + 1) * 128],
                           [pn_.r(c)], [tps.r(c * 2 + blk)])
            for half in H2:
                tps, pT_ = tpsl[half], pT[half]
                if first_blk:
                    for c in range(4):
                        dve(lambda e, pT_=pT_, tps=tps, c=c: e.tensor_copy(out=pT_.ap[:, c * 2 + 1, :], in_=tps.ap[:, c * 2 + 1, :]),
                            [tps.r(c * 2 + 1)], [pT_.r(c * 2 + 1)])
                else:
                    dve(lambda e, pT_=pT_, tps=tps: e.tensor_copy(out=pT_.ap, in_=tps.ap), [tps.r()], [pT_.r()])
            for half in H2:
                pT_ = pT[half]
                for c in range(4):
                    for bi, blk in enumerate(blks):
                        mm(yps.ap[hsl[half], c, :], va.ap[:, tt + blk, half * 64:(half + 1) * 64], pT_.ap[:, c * 2 + blk, :],
                           bi == 0, bi == len(blks) - 1,
                           [va.r(tt + blk), pT_.r(c * 2 + blk)], [yps.r(c)])
            act(yaT.ap[:, :, tt * 128:(tt + 1) * 128], yps.ap, AF.Copy, [yps.r()], [yaT.r()])

        swa_scores(0)
        swa_chain_a(0)
        for tt in range(NTT):
            if tt + 1 < NTT:
                swa_scores(tt + 1)
            swa_chain_b(tt)
            if tt + 1 < NTT:
                swa_chain_a(tt + 1)
        dve(lambda e: e.tensor_copy(out=kaT.ap[:, 0:128], in_=kaT.ap[:, T:T + 128]),
            [kaT.r(lo=T, hi=T + 128)], [kaT.r(lo=0, hi=128)])
        dve(lambda e: e.tensor_copy(out=va.ap[:, 0, :], in_=va.ap[:, 4, :]), [va.r(4)], [va.r(0)])

        if first_of_seq:
            dve(lambda e: e.memset(S.ap, 0.0), [], [S.r()])
        TCS = [slice(tt * 128, (tt + 1) * 128) for tt in range(NTT)]
        zps = [psum(tt, 0, F32, (256,)) for tt in range(NTT)]
        bps = [psum(tt, 256, F32, (2, 128)) for tt in range(NTT)]
        trps = [psum(6, tt * 128, BF16, (2, 128)) for tt in range(NTT)]
        dpsl = [psum(4, 0, F32, (2, 128)), psum(5, 0, F32, (2, 128))]
        apsl = [[psum(2 * (tt % 2) + half, 0 if tt < 2 else 256, F32, (2, 128)) for half in range(2)] for tt in range(NTT)]
        opsl = [[psum(4 + 2 * (tt % 2) + half, 0 if tt < 2 else 256, F32, (2, 128)) for half in range(2)] for tt in range(NTT)]
        for tt in range(NTT):
            mm(zps[tt].ap, alrT.ap[0:32, TCS[tt]], waugv, True, True, [alrT.r(), R_WAUG], [zps[tt].r()])
        for tt in range(NTT):
            act(spb4[tt].ap, zps[tt].ap, AF.Exp, [zps[tt].r()], [spb4[tt].r()], scale=-1.0)
            act(spb4[tt].ap, spb4[tt].ap, AF.Ln, [spb4[tt].r()], [spb4[tt].r()], bias=1.0)
        for tt in range(NTT):
            for fc in range(2):
                mm(bps[tt].ap[:, fc, :], spb4[tt].ap[:, fc * 128:(fc + 1) * 128], triv, True, True,
                   [spb4[tt].r(), R_TRI], [bps[tt].r(fc)])
        for tt in range(NTT):
            dve(lambda e, tt=tt: e.tensor_copy(out=blast4[tt].ap, in_=bps[tt].ap[:, :, 63:128:64]),
                [bps[tt].r()], [blast4[tt].r()])
        for tt in range(NTT):
            act(eb4[tt].ap, bps[tt].ap, AF.Exp, [bps[tt].r()], [eb4[tt].r()])
            act(enb4[tt].ap, bps[tt].ap, AF.Exp, [bps[tt].r()], [enb4[tt].r()], scale=-1.0)
            act(decay4[tt].ap, blast4[tt].ap, AF.Exp, [blast4[tt].r()], [decay4[tt].r()])
        for tt in range(NTT):
            tcs = TCS[tt]
            dve(lambda e, tcs=tcs, tt=tt: e.scalar_tensor_tensor(out=qdec.ap[:, :, tcs], in0=qbT.ap[:, :, tcs], scalar=0.125,
                                                                 in1=eb4[tt].ap, op0=ALU.mult, op1=ALU.mult),
                [qbT.r(), eb4[tt].r()], [qdec.r(0, lo=tt * 128, hi=tt * 128 + 128), qdec.r(1, lo=tt * 128, hi=tt * 128 + 128)])
            dve(lambda e, tcs=tcs, tt=tt: e.tensor_tensor(out=kin.ap[:, :, tcs], in0=kbT.ap[:, :, tcs], in1=enb4[tt].ap, op=ALU.mult),
                [kbT.r(), enb4[tt].r()], [kin.r(0, lo=tt * 128, hi=tt * 128 + 128), kin.r(1, lo=tt * 128, hi=tt * 128 + 128)])
            for fc in range(2):
                for sub in range(2):
                    cs_ = slice(tt * 128 + sub * 64, tt * 128 + (sub + 1) * 64)
                    dve(lambda e, tt=tt, fc=fc, sub=sub, cs_=cs_: e.scalar_tensor_tensor(
                        out=kst.ap[:, fc, cs_], in0=kbT.ap[:, fc, cs_], scalar=decay4[tt].ap[:, fc, sub:sub + 1],
                        in1=enb4[tt].ap[:, fc, sub * 64:(sub + 1) * 64], op0=ALU.mult, op1=ALU.mult),
                        [kbT.r(fc), decay4[tt].r(), enb4[tt].r(fc)],
                        [kst.r(fc, lo=tt * 128 + sub * 64, hi=tt * 128 + (sub + 1) * 64)])
        for tt in range(NTT):
            for fc in range(2):
                tp(trps[tt].ap[:, fc, :], kst.ap[:, fc, TCS[tt]], [kst.r(fc, lo=tt * 128, hi=tt * 128 + 128)], [trps[tt].r(fc)])
        for tt in range(NTT):
            act(kst_tok.ap[:, tt, :], trps[tt].ap.rearrange("p a b -> p (a b)"), AF.Copy, [trps[tt].r()], [kst_tok.r(tt)])
        for tt in range(NTT):
            for sub in range(2):
                sc = tt * 2 + sub
                dps = dpsl[sub]
                ss = slice(sub * 64, (sub + 1) * 64)
                for h in range(4):
                    fc, half = h // 2, h % 2
                    mm(dps.ap[half * 64:(half + 1) * 64, fc, :], kst_tok.ap[ss, tt, h * 64:(h + 1) * 64],
                       vb.ap[ss, tt, h * 128:(h + 1) * 128], True, True,
                       [kst_tok.r(tt), vb.r(tt)], [dps.r(fc)])
                S_src, S_dst = (S, S_alt) if sc % 2 == 0 else (S_alt, S)
                act(Sbf.ap[:, sc, :, :], S_src.ap, AF.Copy, [S_src.r()], [Sbf.r(sc)])
                for fc in range(2):
                    dve(lambda e, fc=fc, sub=sub, dps=dps, tt=tt, S_src=S_src, S_dst=S_dst: e.scalar_tensor_tensor(
                        out=S_dst.ap[:, fc, :], in0=S_src.ap[:, fc, :], scalar=decay4[tt].ap[:, fc, sub:sub + 1],
                        in1=dps.ap[:, fc, :], op0=ALU.mult, op1=ALU.add),
                        [S_src.r(fc), decay4[tt].r(), dps.r(fc)], [S_dst.r(fc)])
        for tt in range(NTT):
            tcs = TCS[tt]
            for h in range(4):
                fc, half = h // 2, h % 2
                hs = slice(half * 64, (half + 1) * 64)
                mm(apsl[tt][half].ap[:, fc, :], kin.ap[hs, fc, tcs], qdec.ap[hs, fc, tcs], True, True,
                   [kin.r(fc, lo=tt * 128, hi=tt * 128 + 128), qdec.r(fc, lo=tt * 128, hi=tt * 128 + 128)],
                   [apsl[tt][half].r(fc)])
        for tt in range(NTT):
            at_ = attnT4[tt]
            for half in range(2):
                for fc in range(2):
                    h = fc * 2 + half
                    dve(lambda e, half=half, fc=fc, h=h, at_=at_, tt=tt: e.tensor_tensor(
                        out=at_.ap[:, h, :], in0=apsl[tt][half].ap[:, fc, :], in1=cmaskv, op=ALU.mult),
                        [apsl[tt][half].r(fc), R_CMASK], [at_.r(h)])
        for tt in range(NTT):
            at_ = attnT4[tt]
            for h in range(4):
                fc, half = h // 2, h % 2
                hs = slice(half * 64, (half + 1) * 64)
                op_ = opsl[tt][half]
                mm(op_.ap[:, fc, :], vb.ap[:, tt, h * 128:(h + 1) * 128], at_.ap[:, h, :], True, False,
                   [vb.r(tt), at_.r(h)], [op_.r(fc)])
                for sub in range(2):
                    sc = tt * 2 + sub
                    mm(op_.ap[:, fc, sub * 64:(sub + 1) * 64], Sbf.ap[hs, sc, fc, :],
                       qdec.ap[hs, fc, tt * 128 + sub * 64:tt * 128 + (sub + 1) * 64], False, sub == 1,
                       [Sbf.r(sc), qdec.r(fc, lo=tt * 128, hi=tt * 128 + 128)], [op_.r(fc)])
        for tt in range(NTT):
            for h in range(4):
                fc, half = h // 2, h % 2
                act(oT.ap[:, h, TCS[tt]], opsl[tt][half].ap[:, fc, :], AF.Copy, [opsl[tt][half].r(fc)],
                    [oT.r(h, lo=tt * 128, hi=tt * 128 + 128)])
        sq4 = Buf("sb", sb, sq.off, BF16, (4, T))
        for h in range(4):
            act(sq4.ap[:, h, :], oT.ap[:, h, :], AF.Square, [oT.r(h)], [sq.r(h)])
        for h in range(4):
            mps = psum(4 + h % 4, 0, F32, (T,))
            mm(mps.ap, onesB.ap, sq4.ap[:, h, :], True, True, [onesB.r(), sq.r(h)], [mps.r()])
            act(rstd4.ap[:, h, :], mps.ap, AF.Ln, [mps.r(), R_EPS], [rstd4.r(h)], bias=epsv)
            act(rstd4.ap[:, h, :], rstd4.ap[:, h, :], AF.Exp, [rstd4.r(h)], [rstd4.r(h)], scale=-0.5)
            dve(lambda e, h=h: e.scalar_tensor_tensor(out=oT.ap[:, h, :], in0=oT.ap[:, h, :],
                                                      scalar=cview[:, C_GLAG:C_GLAG + 1], in1=rstd4.ap[:, h, :],
                                                      op0=ALU.mult, op1=ALU.mult),
                [oT.r(h), R_GAIN, rstd4.r(h)], [oT.r(h)])
            dve(lambda e, h=h: e.tensor_tensor(out=ybT.ap[:, h, :], in0=oT.ap[:, h, :], in1=rbT.ap[:, h, :], op=ALU.mult),
                [oT.r(h), rbT.r(h)], [ybT.r(h)])

        for m in range(8):
            wt = wnext("m%d" % m)
            wpa = wt.ap[:, 0:512].rearrange("p (c n) -> p c n", c=4)
            wpb = wt.ap[:, 512:1024].rearrange("p (c n) -> p c n", c=4)
            wga = wt.ap[:, 1024:2048].rearrange("p (k n) -> p k n", k=8)
            wgb = wt.ap[:, 2048:3072].rearrange("p (k n) -> p k n", k=8)
            par = m % 2
            pa = psum(0 + par * 4, 0, F32, (T,))
            pbb = psum(1 + par * 4, 0, F32, (T,))
            ga = psum(2 + par * 4, 0, F32, (T,))
            gb = psum(3 + par * 4, 0, F32, (T,))
            for k in range(8):
                mm(ga.ap, wga[:, k, :], hT.ap[:, k, :], k == 0, k == 7, [wt.r(), hT.r(k)], [ga.r()])
            for k in range(8):
                mm(gb.ap, wgb[:, k, :], hT.ap[:, k, :], k == 0, k == 7, [wt.r(), hT.r(k)], [gb.r()])
            for c in range(4):
                mm(pa.ap, wpa[:, c, :], yaT.ap[:, c, :], c == 0, c == 3, [wt.r(), yaT.r(c)], [pa.r()])
            for c in range(4):
                mm(pbb.ap, wpb[:, c, :], ybT.ap[:, c, :], c == 0, c == 3, [wt.r(), ybT.r(c)], [pbb.r()])
            act(sga[par].ap, ga.ap, AF.Sigmoid, [ga.r()], [sga[par].r()])
            act(sgb[par].ap, gb.ap, AF.Sigmoid, [gb.r()], [sgb[par].r()])
            dve(lambda e, par=par, pa=pa: e.tensor_tensor(out=t1[par].ap, in0=sga[par].ap, in1=pa.ap, op=ALU.mult),
                [sga[par].r(), pa.r()], [t1[par].r()])
            dve(lambda e, par=par, pbb=pbb: e.tensor_tensor(out=t2[par].ap, in0=sgb[par].ap, in1=pbb.ap, op=ALU.mult),
                [sgb[par].r(), pbb.r()], [t2[par].r()])
            dve(lambda e, par=par, m=m: e.tensor_tensor(out=mgT.ap[:, m, :], in0=t1[par].ap, in1=t2[par].ap, op=ALU.add),
                [t1[par].r(), t2[par].r()], [mgT.r(m)])
            wrefill()
        act(dummy.ap[:, 0:1], epsv, AF.Ln, [R_EPS], [dummy.r()])
        for half in range(2):
            wt = wnext("o%d" % half)
            wv = wt.ap.rearrange("p (k n) -> p k n", k=8)
            for i in range(4):
                m = half * 4 + i
                fp = psum(i if half == 0 else 4 + i, 0, F32, (T,))
                for k in range(8):
                    mm(fp.ap, wv[:, k, i * 128:(i + 1) * 128], mgT.ap[:, k, :], k == 0, k == 7,
                       [wt.r(), mgT.r(k)], [fp.r()])
                act(sq.ap[:, m, :], fp.ap, AF.Square, [fp.r()], [sq.r(m)])
                dve(lambda e, m=m, fp=fp: e.tensor_scalar(out=fT.ap[:, m, :], in0=fp.ap,
                                                          scalar1=agc.ap[:, AGI[3], m:m + 1], scalar2=None, op0=ALU.mult),
                    [fp.r(), agc.r()], [fT.r(m)])
            wrefill()
        postnorm_finish(Xb, 3, 1.0)

    def xload(ci):
        Xn = X[ci % 2]
        tr.add("sp", lambda e, ci=ci, Xn=Xn: e.dma_start(out=Xn.ap.rearrange("p a b -> p (a b)"), in_=xh[ci]),
               [], [Xn.r()], dma="D_x%d" % (ci % 2))

    xload(0)
    pre_done = False
    pending_post = None
    for ci in range(nch):
        Xb = X[ci % 2]
        if "ffn1" in stages:
            ffn(Xb, "f1", 0, 1, skip_prenorm=pre_done, drip=pending_post)
        elif pending_post:
            while pending_post:
                pending_post.pop(0)()
        pending_post = None
        if ci + 1 < nch:
            xload(ci + 1)
        pre_done = False
        if "mix" in stages:
            mixer(Xb, ci)

        def store(ci=ci, Xb=Xb):
            tr.add("sp", lambda e: e.dma_start(out=yh[ci], in_=Xb.ap.rearrange("p a b -> p (a b)")),
                   [Xb.r()], [], dma="D_y%d" % (ci % 2))
        if "ffn2" in stages:
            hook = None
            last = not (ci + 1 < nch and "ffn1" in stages and CUT > 3)
            if not last:
                hook = (lambda ci=ci: prenorm(X[(ci + 1) % 2], 0, bank=3))
                pre_done = True
            r_ = ffn(Xb, "f2", 4, 5, mid_hook=hook, defer_post=not last)
            if r_ is not None:
                pending_post = r_ + [store]
            else:
                store()
        else:
            store()
    final_waits = [("D_y0", tr.dma_count.get("D_y0", 0)), ("D_y1", tr.dma_count.get("D_y1", 0))]

    tr.finalize()
    sem_names = ["E_" + e for e in Tracker.ENGS] + sorted(tr.dma_count.keys())
    sems = {}
    import contextlib
    with contextlib.ExitStack() as es_:
        for n in sem_names:
            sems[n] = es_.enter_context(nc.semaphore(n))
        block = es_.enter_context(nc.Block())

        @block.sync
        def _(e):
            tr.emit("sp", e, sems)
            for n, v in final_waits:
                if v > 0:
                    e.wait_ge(sems[n], v)

        @block.gpsimd
        def _(e):
            tr.emit("pool", e, sems)

        @block.tensor
        def _(e):
            tr.emit("pe", e, sems)

        @block.scalar
        def _(e):
            tr.emit("act", e, sems)

        @block.vector
        def _(e):
            tr.emit("dve", e, sems)
    n_ops = {e: len(tr.ops[e]) for e in Tracker.ENGS}
    print("ops per engine:", n_ops)
    return nc


def _bucket(dist):
    n = np.clip(dist, 0, 127)
    max_exact = 16
    nf = np.maximum(n, 1).astype(np.float32)
    large = max_exact + (np.log(nf / np.float32(max_exact)) / np.float32(math.log(128 / max_exact))
                         * np.float32(32 - max_exact)).astype(np.int32)
    large = np.minimum(large, 31)
    return np.where(n < max_exact, n, large)


def pack_weights(inp):
    tiles = np.zeros((NPT, 128, TILE), np.float32)
    names = [n for n, _ in PASS_TILES]

    def put(name, arr):
        a = np.ascontiguousarray(arr).reshape(128, -1)
        tiles[names.index(name), :, :a.shape[1]] = a

    for pfx, key in (("f1", "ffn1"), ("f2", "ffn2")):
        Wg = inp[key + "_w_gate"][0]
        Wu = inp[key + "_w_up"][0]
        Wd = inp[key + "_w_down"][0]
        Wg_r = Wg.reshape(8, 128, NJ, 128).transpose(2, 1, 0, 3)
        Wu_r = Wu.reshape(8, 128, NJ, 128).transpose(2, 1, 0, 3)
        GU = np.stack([Wg_r, Wu_r], axis=2)
        for i in range(11):
            t_ = GU[2 * i:2 * i + 2].transpose(1, 0, 2, 3, 4)
            put(pfx + "gu%d" % i, t_)
        Wd_r = Wd.reshape(NJ, 128, 2, 512)
        for half in range(2):
            for ti in range(3):
                js = list(range(ti * 8, min(NJ, ti * 8 + 8)))
                t_ = Wd_r[js, :, half, :].transpose(1, 0, 2)
                put(pfx + "d%d" % (half * 3 + ti), t_)
    w_in = inp["w_in"][0]
    qperm = np.array([(half * 4 + c) * 64 + d for c in range(4) for half in range(2) for d in range(64)])

    def kmaj(cols):
        return cols.reshape(8, 128, -1).transpose(1, 0, 2)

    put("a0", kmaj(w_in[:, 0:512][:, qperm]))
    put("a1", kmaj(np.concatenate([w_in[:, 512:640], w_in[:, 640:768], w_in[:, 768:1024]], axis=1)))
    put("a2", kmaj(w_in[:, 1280:1792]))
    put("a3", kmaj(w_in[:, 1792:2304]))
    a4 = np.zeros((1024, 512), np.float32)
    a4[:, 0:256] = w_in[:, 1024:1280]
    a4[:, 256:272] = w_in[:, 2304:2320]
    put("a4", kmaj(a4))
    wpa = inp["w_proj_a"][0][qperm, :]
    wpb = inp["w_proj_b"][0]
    wga = w_in[:, 2320:3344]
    wgb = w_in[:, 3344:4368]
    for m in range(8):
        cs = slice(m * 128, (m + 1) * 128)
        parts = [wpa[:, cs].reshape(4, 128, 128).transpose(1, 0, 2).reshape(128, -1),
                 wpb[:, cs].reshape(4, 128, 128).transpose(1, 0, 2).reshape(128, -1),
                 wga[:, cs].reshape(8, 128, 128).transpose(1, 0, 2).reshape(128, -1),
                 wgb[:, cs].reshape(8, 128, 128).transpose(1, 0, 2).reshape(128, -1)]
        put("m%d" % m, np.concatenate(parts, axis=1))
    wo = inp["w_out"][0]
    for half in range(2):
        put("o%d" % half, kmaj(wo[:, half * 512:(half + 1) * 512]))
    return tiles


def pack_consts(inp):
    c = np.zeros((128, C_TOT), np.float32)
    gl = [inp["ffn1_pre_g"][0], inp["ffn1_post_g"][0], inp["mix_pre_g"][0], inp["mix_post_g"][0],
          inp["ffn2_pre_g"][0], inp["ffn2_post_g"][0]]
    for n, g in enumerate(gl):
        c[:, C_GAIN + n * 8:C_GAIN + (n + 1) * 8] = g.reshape(8, 128).T
    c[:, C_GLAG] = inp["gla_norm_g"][0]
    c[:, C_SINK:C_SINK + 8] = np.broadcast_to(inp["attn_sinks"][0][None, :], (128, 8))
    t_idx = np.arange(128)[:, None]
    j_idx = np.arange(256)[None, :]
    dist = t_idx + 128 - j_idx
    bk = _bucket(dist)
    bias = inp["rel_bias"][bk]
    c[:, C_BIAS:C_BIAS + 2048] = bias.transpose(0, 2, 1).reshape(128, 2048)
    valid = (dist >= 0) & (dist < 128)
    c[:, C_MASK:C_MASK + 256] = np.where(valid, 0.0, -1e30).astype(np.float32)
    c[:, C_IDENT:C_IDENT + 128] = np.eye(128, dtype=np.float32)
    s_i = np.arange(128)[:, None]
    t_i = np.arange(128)[None, :]
    same = (s_i // 64) == (t_i // 64)
    c[:, C_TRI:C_TRI + 128] = np.where(same & (s_i <= t_i), -1.0 / 16.0, 0.0)
    c[:, C_CMASK:C_CMASK + 128] = np.where(same & (s_i <= t_i), 1.0, 0.0)
    c[0:16, C_WAUG:C_WAUG + 256] = inp["w_alpha"][0]
    c[16, C_WAUG:C_WAUG + 256] = inp["b_alpha"][0]
    c[:, C_EPS] = EPS
    return c


_CACHE = {}


def kernel(**inputs):
    inp = {k: np.asarray(v) for k, v in inputs.items()}
    stages = tuple(os.environ.get("MK_STAGES", "ffn1,mix,ffn2").split(","))
    x = inp["x"]
    xr = x.reshape(NCORES, 2, 4, T, 8, 128)
    xh = np.ascontiguousarray(xr.transpose(0, 1, 2, 5, 4, 3)).reshape(NCORES, NCH, 128, 8 * T)
    wt = pack_weights(inp)
    cs = pack_consts(inp)
    nc = build_program(stages)
    in_maps = [{"xh": xh[i], "wst": wt, "cst": cs} for i in range(NCORES)]
    res = run_bass_kernel_spmd(nc, in_maps, core_ids=list(range(NCORES)))
    yh = np.stack([np.asarray(r["yh"]) for r in res.results], axis=0)
    y = yh.reshape(NCORES, 2, 4, 128, 8, T).transpose(0, 1, 2, 5, 4, 3).reshape(16, 2048, 1024)
    return np.ascontiguousarray(y.astype(np.float32))
```
